# Optimizing a Trainium2 kernel written in Bass

```python
import math
import jax
import jax.numpy as jnp
from jax import lax
import numpy as np


D_MODEL = 4096
BATCH = 2
SEQ = 8192
DEPTH = 2

HEAD_DIM = 128
SBA_HEADS = D_MODEL // 256
GDN_HEADS = D_MODEL // 256
GDN_DK = 128
GDN_DV = 128
GDN_CHUNK = 64
CONV_K = 4
DIFF_HEADS = D_MODEL // 256
DIFF_DH = 64
Q_BLOCK = 128
ROPE_THETA = 10000.0
N_GROUPS = 4
EXPERTS_PER_GROUP = 8
N_EXPERTS = N_GROUPS * EXPERTS_PER_GROUP
TOP_K = 2
D_EXPERT = D_MODEL // 8
LN_EPS = 1e-5
NORM_EPS = 1e-6
DEEPNORM_ALPHA = (2 * DEPTH) ** 0.25
DEEPNORM_BETA = (8 * DEPTH) ** -0.25

SBA_W = SBA_HEADS * HEAD_DIM
GDN_QK_W = GDN_HEADS * GDN_DK
GDN_W = GDN_HEADS * GDN_DV
GDN_CONV_CH = 2 * GDN_QK_W + GDN_W
DIFF_W = DIFF_HEADS * 2 * DIFF_DH
IN_SPLIT_SIZES = (3 * SBA_W, GDN_CONV_CH, GDN_W, GDN_HEADS, GDN_HEADS, 3 * DIFF_W, 3 * D_MODEL)
IN_SPLIT_POINTS = tuple(sum(IN_SPLIT_SIZES[:i + 1]) for i in range(len(IN_SPLIT_SIZES) - 1))
N_IN = sum(IN_SPLIT_SIZES)

kernel_name = 'hybrid_sba_gdn_diff_hmoe_deepnorm'


def layer_norm(x, g, b):
    xf = x.astype(jnp.float32)
    mu = jnp.mean(xf, axis=-1, keepdims=True)
    var = jnp.mean(jnp.square(xf - mu), axis=-1, keepdims=True)
    y = (xf - mu) * lax.rsqrt(var + LN_EPS) * g.astype(jnp.float32) + b.astype(jnp.float32)
    return y.astype(x.dtype)


def rms_norm(x, w, eps):
    xf = x.astype(jnp.float32)
    return xf * lax.rsqrt(jnp.mean(xf * xf, axis=-1, keepdims=True) + eps) * w.astype(jnp.float32)


def l2_normalize(x):
    return x * lax.rsqrt(jnp.sum(x * x, axis=-1, keepdims=True) + NORM_EPS)


def rope_tables(seq, dim):
    pos = jnp.arange(seq, dtype=jnp.float32)
    inv_freq = ROPE_THETA ** (-jnp.arange(0, dim, 2, dtype=jnp.float32) / dim)
    ang = pos[:, None] * inv_freq[None, :]
    ang = jnp.concatenate([ang, ang], axis=-1)
    return jnp.cos(ang), jnp.sin(ang)


def apply_rope(x, cos, sin):
    xf = x.astype(jnp.float32)
    half = xf.shape[-1] // 2
    rot = jnp.concatenate([-xf[..., half:], xf[..., :half]], axis=-1)
    c = cos[None, None, :, None, :]
    s = sin[None, None, :, None, :]
    return (xf * c + rot * s).astype(x.dtype)


def to_query_blocks(t):
    b, h, s = t.shape[:3]
    t = t.reshape((b, h, s // Q_BLOCK, Q_BLOCK) + t.shape[3:])
    return jnp.moveaxis(t, 2, 0)


def from_query_blocks(o):
    nb, b, h, q, dv = o.shape
    return o.transpose(1, 0, 3, 2, 4).reshape(b, nb * q, h * dv)


def stick_breaking_attention(q, k, v):
    seq = q.shape[2]
    scale = HEAD_DIM ** -0.5
    key_pos = jnp.arange(seq)

    def block(args):
        q_blk, blk = args
        z = jnp.einsum('bhqd,bhkd->bhqk', q_blk, k).astype(jnp.float32) * scale
        q_pos = blk * Q_BLOCK + jnp.arange(Q_BLOCK)
        strict = key_pos[None, :] < q_pos[:, None]
        log_1m_beta = jnp.where(strict, jax.nn.log_sigmoid(-z), 0.0)
        later = lax.cumsum(log_1m_beta, axis=3, reverse=True) - log_1m_beta
        att = jnp.where(strict, jnp.exp(jax.nn.log_sigmoid(z) + later), 0.0)
        return jnp.einsum('bhqk,bhkd->bhqd', att.astype(v.dtype), v)

    out = lax.map(block, (to_query_blocks(q), jnp.arange(seq // Q_BLOCK)))
    return from_query_blocks(out)


def differential_attention(q, k, v, lam, subln_w, lam_init):
    seq = q.shape[2]
    scale = DIFF_DH ** -0.5
    key_pos = jnp.arange(seq)

    def block(args):
        q_blk, blk = args
        s = jnp.einsum('bhqcd,bhkcd->bhcqk', q_blk, k).astype(jnp.float32) * scale
        q_pos = blk * Q_BLOCK + jnp.arange(Q_BLOCK)
        causal = key_pos[None, :] <= q_pos[:, None]
        p = jax.nn.softmax(jnp.where(causal, s, -jnp.inf), axis=-1)
        att = p[:, :, 0] - lam * p[:, :, 1]
        return jnp.einsum('bhqk,bhkd->bhqd', att.astype(v.dtype), v)

    out = lax.map(block, (to_query_blocks(q), jnp.arange(seq // Q_BLOCK)))
    out = rms_norm(out, subln_w, LN_EPS) * (1.0 - lam_init)
    return from_query_blocks(out.astype(v.dtype))


def causal_depthwise_conv(x, w):
    seq = x.shape[1]
    xp = jnp.pad(x, ((0, 0), (CONV_K - 1, 0), (0, 0)))
    y = xp[:, 0:seq, :] * w[0]
    for j in range(1, CONV_K):
        y = y + xp[:, j:j + seq, :] * w[j]
    return y


def chunk_gated_delta_rule(q, k, v, g, beta):
    bsz, seq, h, dk = q.shape
    dv = v.shape[-1]
    c = GDN_CHUNK
    n = seq // c

    def chunks(t):
        t = t.reshape((bsz, n, c, h) + t.shape[3:])
        return jnp.moveaxis(t, 3, 1)

    q, k, v = chunks(q), chunks(k), chunks(v)
    g = jnp.cumsum(chunks(g), axis=-1)
    beta = chunks(beta)
    idx = jnp.arange(c)
    incl = idx[:, None] >= idx[None, :]
    strict = idx[:, None] > idx[None, :]
    gdiff = g[..., :, None] - g[..., None, :]
    decay = jnp.where(incl, jnp.exp(jnp.where(incl, gdiff, 0.0)), 0.0)
    k_beta = k * beta[..., None]
    lower = jnp.where(strict, jnp.einsum('bhncd,bhnkd->bhnck', k_beta, k) * decay, 0.0)
    system = lower + jnp.eye(c, dtype=jnp.float32)
    u = lax.linalg.triangular_solve(system, v * beta[..., None], left_side=True, lower=True, unit_diagonal=True)
    w = lax.linalg.triangular_solve(system, k_beta * jnp.exp(g)[..., None], left_side=True, lower=True, unit_diagonal=True)
    intra = jnp.einsum('bhncd,bhnkd->bhnck', q, k) * decay
    q_dec = q * jnp.exp(g)[..., None]
    k_dec = k * jnp.exp(g[..., -1:] - g)[..., None]
    chunk_decay = jnp.exp(g[..., -1])

    def step(state, xs):
        u_c, w_c, q_c, k_c, a_c, d_c = xs
        v_new = u_c - jnp.einsum('bhck,bhkv->bhcv', w_c, state)
        o_c = jnp.einsum('bhck,bhkv->bhcv', q_c, state) + jnp.einsum('bhcs,bhsv->bhcv', a_c, v_new)
        state = state * d_c[..., None, None] + jnp.einsum('bhck,bhcv->bhkv', k_c, v_new)
        return state, o_c

    xs = tuple(jnp.moveaxis(t, 2, 0) for t in (u, w, q_dec, k_dec, intra, chunk_decay))
    state0 = jnp.zeros((bsz, h, dk, dv), jnp.float32)
    _, o = lax.scan(step, state0, xs)
    return o.transpose(1, 0, 3, 2, 4).reshape(bsz, seq, h, dv)


def gated_deltanet(qkv, z, a, b, conv_w, a_log, dt_bias, norm_w):
    bsz, seq, _ = qkv.shape
    out_dtype = qkv.dtype
    qkv = jax.nn.silu(causal_depthwise_conv(qkv, conv_w)).astype(jnp.float32)
    q, k, v = jnp.split(qkv, [GDN_QK_W, 2 * GDN_QK_W], axis=-1)
    q = l2_normalize(q.reshape(bsz, seq, GDN_HEADS, GDN_DK)) * (GDN_DK ** -0.5)
    k = l2_normalize(k.reshape(bsz, seq, GDN_HEADS, GDN_DK))
    v = v.reshape(bsz, seq, GDN_HEADS, GDN_DV)
    beta = jax.nn.sigmoid(b.astype(jnp.float32))
    g = -jnp.exp(a_log.astype(jnp.float32)) * jax.nn.softplus(a.astype(jnp.float32) + dt_bias.astype(jnp.float32))
    o = chunk_gated_delta_rule(q, k, v, g, beta)
    zf = z.astype(jnp.float32).reshape(bsz, seq, GDN_HEADS, GDN_DV)
    o = rms_norm(o, norm_w, NORM_EPS) * jax.nn.silu(zf)
    return o.reshape(bsz, seq, GDN_W).astype(out_dtype)


def hybrid_mixer(x, w_in, conv_w, gdn_a_log, gdn_dt_bias, gdn_norm_w, lq1, lk1, lq2, lk2, subln_w,
                 w_branch_sba, w_branch_gdn, w_branch_diff, w_out, cos, sin, layer):
    bsz, seq, _ = x.shape
    proj = jnp.einsum('bsd,dn->bsn', x, w_in)
    sba_qkv, gdn_qkv, gdn_z, gdn_a, gdn_b, diff_qkv, gate_logits = jnp.split(proj, IN_SPLIT_POINTS, axis=-1)

    def heads(t, h, d):
        return t.reshape(bsz, seq, h, d).transpose(0, 2, 1, 3)
    sq, sk, sv = jnp.split(sba_qkv, 3, axis=-1)
    y_sba = stick_breaking_attention(heads(sq, SBA_HEADS, HEAD_DIM), heads(sk, SBA_HEADS, HEAD_DIM),
                                     heads(sv, SBA_HEADS, HEAD_DIM))

    y_gdn = gated_deltanet(gdn_qkv, gdn_z, gdn_a, gdn_b, conv_w, gdn_a_log, gdn_dt_bias, gdn_norm_w)

    dq, dk, dv = jnp.split(diff_qkv, 3, axis=-1)
    dq = dq.reshape(bsz, seq, DIFF_HEADS, 2, DIFF_DH).transpose(0, 2, 1, 3, 4)
    dk = dk.reshape(bsz, seq, DIFF_HEADS, 2, DIFF_DH).transpose(0, 2, 1, 3, 4)
    dv = heads(dv, DIFF_HEADS, 2 * DIFF_DH)
    lam_init = 0.8 - 0.6 * math.exp(-0.3 * layer)
    lam = (jnp.exp(jnp.sum(lq1.astype(jnp.float32) * lk1.astype(jnp.float32)))
           - jnp.exp(jnp.sum(lq2.astype(jnp.float32) * lk2.astype(jnp.float32))) + lam_init)
    y_diff = differential_attention(apply_rope(dq, cos, sin), apply_rope(dk, cos, sin), dv, lam, subln_w, lam_init)

    g_sba, g_gdn, g_diff = jnp.split(jax.nn.sigmoid(gate_logits), 3, axis=-1)
    merged = (g_sba * jnp.einsum('bsw,wd->bsd', y_sba, w_branch_sba)
              + g_gdn * jnp.einsum('bsw,wd->bsd', y_gdn, w_branch_gdn)
              + g_diff * jnp.einsum('bsw,wd->bsd', y_diff, w_branch_diff))
    return jnp.einsum('bsd,de->bse', merged, w_out)


def hierarchical_moe(x, w_rg, b_rg, w_re, b_re, w_gate, w_up, w_down):
    bsz, seq, d = x.shape
    t = bsz * seq
    xt = x.reshape(t, d)
    p_group = jax.nn.softmax(jnp.einsum('td,dg->tg', xt, w_rg).astype(jnp.float32) + b_rg.astype(jnp.float32), axis=-1)
    g_w, g_idx = lax.top_k(p_group, 1)
    e_logits = (jnp.einsum('td,de->te', xt, w_re).astype(jnp.float32) + b_re.astype(jnp.float32))
    e_logits = e_logits.reshape(t, N_GROUPS, EXPERTS_PER_GROUP)
    sel = e_logits[jnp.arange(t), g_idx[:, 0]]
    e_w, e_idx = lax.top_k(jax.nn.softmax(sel, axis=-1), TOP_K)
    weights = g_w * (e_w / jnp.sum(e_w, axis=-1, keepdims=True))
    expert_id = g_idx * EXPERTS_PER_GROUP + e_idx
    combine = jnp.sum(jax.nn.one_hot(expert_id, N_EXPERTS, dtype=jnp.float32) * weights[..., None], axis=1)
    combine = combine.reshape(t, N_GROUPS, EXPERTS_PER_GROUP)
    out = jnp.zeros((t, d), jnp.float32)
    for gi in range(N_GROUPS):
        sl = slice(gi * EXPERTS_PER_GROUP, (gi + 1) * EXPERTS_PER_GROUP)
        h = jax.nn.silu(jnp.einsum('td,edf->tef', xt, w_gate[sl])) * jnp.einsum('td,edf->tef', xt, w_up[sl])
        h = h * combine[:, gi, :, None].astype(h.dtype)
        out = out + jnp.einsum('tef,efd->td', h, w_down[sl]).astype(jnp.float32)
    return out.reshape(bsz, seq, d).astype(x.dtype)


def setup_inputs(seed: int = 0) -> dict:
    key = jax.random.key(seed)
    ks = iter(jax.random.split(key, 32))
    f32 = jnp.float32

    def nrm(shape, scale):
        return jax.random.normal(next(ks), shape, f32) * scale

    def gain(shape):
        return 1.0 + nrm(shape, 0.02)

    x = nrm((BATCH, SEQ, D_MODEL), 1.0)
    w_in = nrm((DEPTH, D_MODEL, N_IN), D_MODEL ** -0.5)
    conv_w = nrm((DEPTH, CONV_K, GDN_CONV_CH), CONV_K ** -0.5)
    gdn_a_log = jnp.log(jax.random.uniform(next(ks), (DEPTH, GDN_HEADS), f32, 1.0, 16.0))
    dt = jnp.exp(jax.random.uniform(next(ks), (DEPTH, GDN_HEADS), f32, math.log(1e-3), math.log(1e-1)))
    gdn_dt_bias = dt + jnp.log(-jnp.expm1(-dt))
    gdn_norm_w = gain((DEPTH, GDN_DV))
    diff_lambda_q1 = nrm((DEPTH, DIFF_DH), 0.1)
    diff_lambda_k1 = nrm((DEPTH, DIFF_DH), 0.1)
    diff_lambda_q2 = nrm((DEPTH, DIFF_DH), 0.1)
    diff_lambda_k2 = nrm((DEPTH, DIFF_DH), 0.1)
    diff_subln_w = gain((DEPTH, 2 * DIFF_DH))
    w_branch_sba = nrm((DEPTH, SBA_W, D_MODEL), SBA_W ** -0.5 * DEEPNORM_BETA)
    w_branch_gdn = nrm((DEPTH, GDN_W, D_MODEL), GDN_W ** -0.5 * DEEPNORM_BETA)
    w_branch_diff = nrm((DEPTH, DIFF_W, D_MODEL), DIFF_W ** -0.5 * DEEPNORM_BETA)
    w_out = nrm((DEPTH, D_MODEL, D_MODEL), D_MODEL ** -0.5 * DEEPNORM_BETA)
    ln1_g = gain((DEPTH, D_MODEL))
    ln1_b = nrm((DEPTH, D_MODEL), 0.02)
    w_router_group = nrm((DEPTH, D_MODEL, N_GROUPS), D_MODEL ** -0.5)
    b_router_group = nrm((DEPTH, N_GROUPS), 0.01)
    w_router_expert = nrm((DEPTH, D_MODEL, N_EXPERTS), D_MODEL ** -0.5)
    b_router_expert = nrm((DEPTH, N_EXPERTS), 0.01)
    w_expert_gate = nrm((DEPTH, N_EXPERTS, D_MODEL, D_EXPERT), D_MODEL ** -0.5)
    w_expert_up = nrm((DEPTH, N_EXPERTS, D_MODEL, D_EXPERT), D_MODEL ** -0.5)
    w_expert_down = nrm((DEPTH, N_EXPERTS, D_EXPERT, D_MODEL), D_EXPERT ** -0.5 * DEEPNORM_BETA)
    ln2_g = gain((DEPTH, D_MODEL))
    ln2_b = nrm((DEPTH, D_MODEL), 0.02)
    return {'x': x, 'w_in': w_in, 'conv_w': conv_w, 'gdn_a_log': gdn_a_log, 'gdn_dt_bias': gdn_dt_bias,
            'gdn_norm_w': gdn_norm_w, 'diff_lambda_q1': diff_lambda_q1, 'diff_lambda_k1': diff_lambda_k1,
            'diff_lambda_q2': diff_lambda_q2, 'diff_lambda_k2': diff_lambda_k2, 'diff_subln_w': diff_subln_w,
            'w_branch_sba': w_branch_sba, 'w_branch_gdn': w_branch_gdn, 'w_branch_diff': w_branch_diff,
            'w_out': w_out, 'ln1_g': ln1_g, 'ln1_b': ln1_b, 'w_router_group': w_router_group,
            'b_router_group': b_router_group, 'w_router_expert': w_router_expert,
            'b_router_expert': b_router_expert, 'w_expert_gate': w_expert_gate, 'w_expert_up': w_expert_up,
            'w_expert_down': w_expert_down, 'ln2_g': ln2_g, 'ln2_b': ln2_b}


def reference(x, w_in, conv_w, gdn_a_log, gdn_dt_bias, gdn_norm_w, diff_lambda_q1, diff_lambda_k1,
              diff_lambda_q2, diff_lambda_k2, diff_subln_w, w_branch_sba, w_branch_gdn, w_branch_diff,
              w_out, ln1_g, ln1_b, w_router_group, b_router_group, w_router_expert, b_router_expert,
              w_expert_gate, w_expert_up, w_expert_down, ln2_g, ln2_b):
    cos, sin = rope_tables(x.shape[1], DIFF_DH)
    for l in range(DEPTH):
        h = hybrid_mixer(x, w_in[l], conv_w[l], gdn_a_log[l], gdn_dt_bias[l], gdn_norm_w[l],
                         diff_lambda_q1[l], diff_lambda_k1[l], diff_lambda_q2[l], diff_lambda_k2[l],
                         diff_subln_w[l], w_branch_sba[l], w_branch_gdn[l], w_branch_diff[l], w_out[l],
                         cos, sin, l)
        x = layer_norm(DEEPNORM_ALPHA * x + h, ln1_g[l], ln1_b[l])
        h = hierarchical_moe(x, w_router_group[l], b_router_group[l], w_router_expert[l], b_router_expert[l],
                             w_expert_gate[l], w_expert_up[l], w_expert_down[l])
        x = layer_norm(DEEPNORM_ALPHA * x + h, ln2_g[l], ln2_b[l])
    return x
```

```python
import math
import numpy as np
from contextlib import ExitStack
from concourse.bass_utils import run_bass_kernel_spmd
import concourse.bass as bass
import concourse.mybir as mybir
F32 = mybir.dt.float32; BF16 = mybir.dt.bfloat16; I32 = mybir.dt.int32
AF = mybir.ActivationFunctionType
ALU = mybir.AluOpType
AX = mybir.AxisListType

class Buf:
    __slots__ = ("name", "w", "r", "dsem", "dcnt")
    def __init__(self, name):
        self.name = name; self.w = None; self.r = {}; self.dsem = None; self.dcnt = 0

class MK:
    ENGS = ("pe", "act", "dve", "pool", "sp")
    def __init__(self, nc, stack):
        self.nc = nc; self.stack = stack
        self.ops = {e: [] for e in self.ENGS}
        self.sem = {}
        for e in ("pe", "act", "dve", "pool"):
            self.sem[e] = stack.enter_context(nc.semaphore("sem_" + e))
        self.cnt = {e: 0 for e in self.ENGS}
        self.seen = {e: {} for e in self.ENGS}
        self.semobj = dict(self.sem)
        self.nd = 0
        self.all_tokens = []
    def buf(self, name="b"):
        return Buf(name)
    def _need(self, eng, reads, writes, skip_self=False):
        need = {}
        def add(tok):
            if tok is None: return
            k, v = tok
            if skip_self and k == eng: return
            if need.get(k, 0) < v: need[k] = v
        if getattr(self, "serial", False):
            for k in ("pe", "act", "dve", "pool"):
                if self.cnt[k] > 0: add((k, self.cnt[k]))
            for k, v in getattr(self, "dlast", {}).items(): add((k, v))
        for b in reads: add(b.w)
        for b in writes:
            add(b.w)
            for k, v in b.r.items(): add((k, v))
        seen = self.seen[eng]
        for k, v in need.items():
            if seen.get(k, 0) >= v: continue
            seen[k] = v
            so = self.semobj[k]
            self.ops[eng].append(lambda e, so=so, v=v: e.wait_ge(so, v))
    def _done(self, tok, reads, writes):
        k, v = tok
        for b in reads:
            if b.r.get(k, 0) < v: b.r[k] = v
        for b in writes:
            b.w = tok; b.r = {}
    def op(self, eng, fn, reads=(), writes=(), skip_self=False, fence=False):
        self._need(eng, reads, writes, skip_self)
        self.cnt[eng] += 1
        idx = self.cnt[eng]
        so = self.sem[eng]
        self.ops[eng].append(lambda e, fn=fn, so=so: fn(e).then_inc(so, 1))
        if (fence or eng in getattr(self, "fence_engs", ())) and getattr(self, "fence_ap", None) is not None:
            self.cnt[eng] += 1
            idx = self.cnt[eng]
            fa = self.fence_ap
            if eng == "act":
                self.ops[eng].append(lambda e, fa=fa, so=so: e.memzero(fa).then_inc(so, 1))
            else:
                self.ops[eng].append(lambda e, fa=fa, so=so: e.memset(fa, 0.0).then_inc(so, 1))
        self.seen[eng][eng] = max(self.seen[eng].get(eng, 0), 0)
        self._done((eng, idx), reads, writes)
    def do(self, eng, method, *args, reads=(), writes=(), skip_self=False, **kw):
        self.op(eng, lambda e, method=method, args=args, kw=kw: getattr(e, method)(*args, **kw), reads=reads, writes=writes, skip_self=skip_self,
                fence=(eng == "dve" and method == "scalar_tensor_tensor"))
    def dma(self, q, out, in_, reads=(), writes=(), sembuf=None, **kw):
        if sembuf is None:
            sembuf = (list(writes) + list(reads))[0]
        if sembuf.dsem is None:
            self.nd += 1
            sembuf.dsem = "d%d_%s" % (self.nd, sembuf.name)
            self.semobj[sembuf.dsem] = self.stack.enter_context(self.nc.semaphore(sembuf.dsem))
        self._need(q, reads, writes)
        sembuf.dcnt += 16
        so = self.semobj[sembuf.dsem]
        self.ops[q].append(lambda e, out=out, in_=in_, so=so, kw=kw: e.dma_start(out=out, in_=in_, **kw).then_inc(so, 16))
        if not hasattr(self, "dlast"): self.dlast = {}
        self.dlast[sembuf.dsem] = sembuf.dcnt
        self._done((sembuf.dsem, sembuf.dcnt), reads, writes)
        return (sembuf.dsem, sembuf.dcnt)
    def barrier(self):
        toks = [(k, self.cnt[k]) for k in ("pe", "act", "dve", "pool") if self.cnt[k] > 0]
        toks += list(getattr(self, "dlast", {}).items())
        for eng in self.ENGS:
            seen = self.seen[eng]
            for k, v in toks:
                if seen.get(k, 0) >= v: continue
                seen[k] = v
                so = self.semobj[k]
                self.ops[eng].append(lambda e, so=so, v=v: e.wait_ge(so, v))
    def finish(self, final_bufs):
        need = {}
        for b in final_bufs:
            for tok in [b.w] + list(b.r.items()):
                if tok is None: continue
                k, v = tok
                need[k] = max(need.get(k, 0), v)
        for k, v in need.items():
            so = self.semobj[k]
            self.ops["sp"].append(lambda e, so=so, v=v: e.wait_ge(so, v))
        nc = self.nc
        ops = self.ops
        with nc.Block() as block:
            @block.sync
            def _(e):
                for f in ops["sp"]: f(e)
            @block.tensor
            def _(e):
                for f in ops["pe"]: f(e)
            @block.scalar
            def _(e):
                for f in ops["act"]: f(e)
            @block.vector
            def _(e):
                for f in ops["dve"]: f(e)
            @block.gpsimd
            def _(e):
                for f in ops["pool"]: f(e)


TT = 512
LN_EPS = 1e-5
NORM_EPS = 1e-6

def make_cst():
    c = {}
    p = np.arange(128)[:, None]; j = np.arange(128)[None, :]
    c["ident"] = (p == j).astype(np.float32)
    c["ones"] = np.ones((128, 128), np.float32)
    c["negtri"] = -(p >= j).astype(np.float32)
    c["negones"] = -np.ones((128, 128), np.float32)
    jj = np.arange(512)[None, :]
    for i in range(4):
        c["mstrict%d" % i] = ((i * 128 + p) < jj).astype(np.float32)
        c["mincl%d" % i] = ((i * 128 + p) <= jj).astype(np.float32)
    same = (p // 64) == (j // 64)
    NEG = -30000.0
    c["bt"] = (same & (p <= j)).astype(np.float32)
    c["negm_strict"] = np.where(same & (p > j), 0.0, NEG).astype(np.float32)
    c["negm_inclT"] = np.where(same & (j >= p), 0.0, NEG).astype(np.float32)
    fn = ["ident", "ones", "bt", "negm_strict", "negm_inclT"]
    bn = ["ones", "negtri", "negones"] + ["mstrict%d" % i for i in range(4)] + ["mincl%d" % i for i in range(4)]
    def pack(names):
        offs = {}; o = 0
        for n in names:
            offs[n] = (o, c[n].shape[1]); o += c[n].shape[1]
        return np.ascontiguousarray(np.concatenate([c[n] for n in names], axis=1)), offs
    af, of = pack(fn); ab, ob = pack(bn)
    return af, of, ab, ob

def make_rope(S):
    pos = np.arange(S, dtype=np.float32)
    inv_freq = (10000.0 ** (-np.arange(0, 64, 2, dtype=np.float32) / 64)).astype(np.float32)
    ang = pos[None, :] * inv_freq[:, None]
    cos = np.cos(ang).astype(np.float32); sin = np.sin(ang).astype(np.float32)
    cosT = np.concatenate([cos, cos, cos, cos], axis=0)
    sinS = np.concatenate([sin, -sin, sin, -sin], axis=0)
    return cosT, sinS

class L1:
    def __init__(self, S, B, units):
        self.S, self.B, self.units = S, B, units
        self.NT = S // TT; self.NB = S // 128
        self.cstf_np, self.cofff, self.cstb_np, self.coffb = make_cst()
        self.NCF = self.cstf_np.shape[1]; self.NCB = self.cstb_np.shape[1]
        self.kinds = set(k for (k, _, _) in units)

    def build(self):
        S, B = self.S, self.B
        nc = bass.Bass("TRN2", target_bir_lowering=False)
        self.nc = nc
        D = lambda n, s, kind: nc.dram_tensor(n, s, F32, kind=kind).ap()
        self.xT = D("xT", [B, 128, 32, S], "ExternalInput")
        kinds = self.kinds
        if "sba" in kinds:
            self.wsba = D("wsba", [2, 3, 128, 32, 128], "ExternalInput")
            self.ysba = D("ysba", [2, B, 128, S], "ExternalOutput")
        if "diff" in kinds:
            self.wdiff = D("wdiff", [2, 3, 128, 32, 128], "ExternalInput")
            self.ropec = D("ropec", [128, S], "ExternalInput")
            self.ropes = D("ropes", [128, S], "ExternalInput")
            self.ydiff = D("ydiff", [2, B, 128, S], "ExternalOutput")
        if "gdn" in kinds:
            self.wgdn = D("wgdn", [2, 4, 128, 32, 128], "ExternalInput")
            self.wgab = D("wgab", [128, 32, 4], "ExternalInput")
            self.ygdn = D("ygdn", [2, B, S, 128], "ExternalOutput")
        self.vec = D("vec", [128, 1024], "ExternalInput")
        self.cstf = D("cstf", [128, self.NCF], "ExternalInput")
        self.cstb = D("cstb", [128, self.NCB], "ExternalInput")
        with ExitStack() as st:
            self.st = st
            mk = MK(nc, st); self.mk = mk
            self.alloc()
            self.load_consts()
            for (kind, hs, b) in self.units:
                getattr(self, "unit_" + kind)(hs, b)
            for (name, t, tb) in getattr(self, "dbg", []):
                shp = list(t.shape)
                d = nc.dram_tensor("dbg_" + name, shp, F32, kind="ExternalOutput").ap()
                db = mk.buf("dbg_" + name)
                mk.dma("pool", d, t[:], reads=[tb], writes=[db])
                self.outbufs.append(db)
            mk.finish(self.outbufs)
        return nc

    def probe(self, name, t, tb):
        if not getattr(self, "probing", False): return
        shp = list(t.shape)
        d = self.nc.dram_tensor("dbg_" + name, shp, F32, kind="ExternalOutput").ap()
        db = self.mk.buf("dbg_" + name)
        self.mk.dma("pool", d, t[:], reads=[tb], writes=[db])
        self.outbufs.append(db)

    def T(self, name, shape, dt=F32):
        t = self.st.enter_context(self.nc.sbuf_tensor(name, shape, dt))
        return t, self.mk.buf(name)

    def alloc(self):
        nc, mk, st = self.nc, self.mk, self.st
        S, NB = self.S, self.NB
        self.ps = []; self.pb = []
        for i in range(8):
            self.ps.append(st.enter_context(nc.psum_tensor("ps%d" % i, [128, 512], F32)))
            self.pb.append(mk.buf("ps%d" % i))
        self.xt = [self.T("xt%d" % i, [128, 32, TT], BF16) for i in range(1 if self.kinds == {"gdn"} else 2)]
        self.w = [self.T("w%d" % i, [128, 32, 128], BF16) for i in range(4)]
        self.wab = self.T("wab", [128, 32, 4], BF16)
        self.cf = self.T("cf", [128, self.NCF], F32)
        self.cb = self.T("cb", [128, self.NCB], BF16)
        self.vc = self.T("vc", [128, 1024], F32)
        self.sm = self.T("sm", [128, 64], F32)
        self.fence_t = self.T("fence_t", [128, 4], F32)
        mk.fence_ap = self.fence_t[0][:, 0:1]
        import os
        mk.fence_engs = tuple(x for x in os.environ.get("FENCE", "").split(",") if x)
        self.epst = self.T("epst", [128, 4], F32)
        mk.op("dve", lambda e: e.memset(self.epst[0][:, 0:1], LN_EPS), writes=[self.epst[1]])
        mk.op("dve", lambda e: e.memset(self.epst[0][:, 1:2], NORM_EPS), writes=[self.epst[1]])
        self.Ysba = mk.buf("Ysba"); self.Ydiff = mk.buf("Ydiff"); self.Ygdn = mk.buf("Ygdn")
        self.outbufs = [self.Ysba, self.Ydiff, self.Ygdn]
        self.xt_cnt = 0
        if not (self.kinds & {"sba", "diff"}):
            return
        self.QT = self.T("QT", [128, S], BF16)
        self.KT = self.T("KT", [128, S], BF16)
        self.V = self.T("V", [128, NB, 128], BF16)
        self.qtb = [mk.buf("qt%d" % i) for i in range(self.NT)]
        self.ktb = [mk.buf("kt%d" % i) for i in range(self.NT)]
        self.vtb = [mk.buf("vt%d" % i) for i in range(self.NT)]
        self.Et = [self.T("Et%d" % i, [128, TT], F32) for i in range(2)]
        self.SPb = [self.T("SPb%d" % i, [128, TT], BF16) for i in range(2)]
        self.att = [self.T("att%d" % i, [128, TT], BF16) for i in range(3)]
        self.SPacc = self.T("SPacc", [128, TT], F32)
        self.SPaccb = [self.T("SPaccb%d" % i, [128, TT], BF16) for i in range(2)]
        self.ost = [self.T("ost%d" % i, [128, TT], F32) for i in range(2)]
        self.f32t = [self.T("f32t%d" % i, [128, TT], F32) for i in range(6)]
        self.rc = [self.T("rc%d" % i, [128, TT], F32) for i in range(2)]
        self.rs = [self.T("rs%d" % i, [128, TT], F32) for i in range(2)]

    def c_f(self, name, cols=None):
        o, w = self.cofff[name]
        return self.cf[0][:, o:o + (cols or w)]

    def c_b(self, name, cols=None):
        o, w = self.coffb[name]
        return self.cb[0][:, o:o + (cols or w)]

    def load_consts(self):
        mk = self.mk
        mk.dma("sp", self.cf[0][:], self.cstf, writes=[self.cf[1]])
        mk.dma("pool", self.cb[0][:], self.cstb, writes=[self.cb[1]])
        mk.dma("sp", self.vc[0][:], self.vec, writes=[self.vc[1]])

    def load_w(self, src, n):
        for i in range(n):
            self.mk.dma("pool", self.w[i][0][:], src[i], writes=[self.w[i][1]])

    def load_xt(self, b, t):
        s = self.xt_cnt % len(self.xt); self.xt_cnt += 1
        xt, xb = self.xt[s]
        self.mk.dma("pool", xt[:], self.xT[b, :, :, t * TT:(t + 1) * TT], writes=[xb])
        return xt, xb

    def proj_fm(self, xt, xb, wi, bank):
        mk = self.mk; w, wb = self.w[wi]; ps, pb = self.ps[bank], self.pb[bank]
        for k in range(32):
            mk.op("pe", lambda e, k=k: e.matmul(ps[:], w[:, k, :], xt[:, k, :], start=(k == 0), stop=(k == 31)),
                  reads=[wb, xb], writes=[pb], skip_self=True)

    def proj_tm(self, xt, xb, wi, bank):
        mk = self.mk; w, wb = self.w[wi]; ps, pb = self.ps[bank], self.pb[bank]
        for blk in range(4):
            for k in range(32):
                mk.op("pe", lambda e, k=k, blk=blk: e.matmul(ps[:, blk * 128:(blk + 1) * 128], xt[:, k, blk * 128:(blk + 1) * 128], w[:, k, :],
                                                             start=(k == 0), stop=(k == 31)),
                      reads=[wb, xb], writes=[pb], skip_self=True)

    def unit_sba(self, hs, b):
        mk = self.mk; NT = self.NT
        self.load_w(self.wsba[hs], 3)
        scale = 128 ** -0.5
        QT, _ = self.QT; KT, _ = self.KT; V, _ = self.V
        def proj(t):
            xt, xb = self.load_xt(b, t)
            self.proj_fm(xt, xb, 0, 5)
            mk.op("act", lambda e: e.activation(QT[:, t * TT:(t + 1) * TT], self.ps[5][:], AF.Copy, scale=scale), reads=[self.pb[5]], writes=[self.qtb[t]])
            self.proj_fm(xt, xb, 1, 6)
            mk.op("dve", lambda e: e.tensor_copy(KT[:, t * TT:(t + 1) * TT], self.ps[6][:]), reads=[self.pb[6]], writes=[self.ktb[t]])
            self.proj_tm(xt, xb, 2, 7)
            mk.op("dve", lambda e: e.tensor_copy(V[:, 4 * t:4 * t + 4, :], self.ps[7][:].rearrange("p (a d) -> p a d", a=4)), reads=[self.pb[7]], writes=[self.vtb[t]])
        proj(0)
        for qt in range(NT):
            if qt + 1 < NT:
                proj(qt + 1)
            self.sba_attn(hs, b, qt)

    def sba_attn(self, hs, b, qt):
        mk = self.mk
        QT, _ = self.QT; KT, _ = self.KT; V, _ = self.V
        nkb = 4 * qt + 4
        kbs = list(range(nkb - 1, -1, -1))
        qs = slice(qt * TT, (qt + 1) * TT)
        psC, pbC = self.ps[4], self.pb[4]
        SPacc, SPaccB = self.SPacc
        def s1(idx):
            kb = kbs[idx]; a = idx % 2; i = kb - 4 * qt
            psA, pbA = self.ps[a], self.pb[a]
            Et, Etb = self.Et[a]; SPb, SPbb = self.SPb[a]
            ks = slice(kb * 128, (kb + 1) * 128)
            mk.op("pe", lambda e: e.matmul(psA[:], KT[:, ks], QT[:, qs], start=True, stop=True),
                  reads=[self.ktb[kb // 4], self.qtb[qt]], writes=[pbA], skip_self=True)
            mk.op("act", lambda e: e.activation(Et[:], psA[:], AF.Exp), reads=[pbA], writes=[Etb])
            mk.op("act", lambda e: e.activation(SPb[:], Et[:], AF.Ln, bias=1.0), reads=[Etb], writes=[SPbb])
            if i >= 0:
                m = self.c_b("mstrict%d" % i)
                mk.op("dve", lambda e: e.tensor_tensor(SPb[:], SPb[:], m, ALU.mult), reads=[SPbb, self.cb[1]], writes=[SPbb])
        def s2(idx):
            kb = kbs[idx]; a = idx % 2; i = kb - 4 * qt
            psB, pbB = self.ps[2 + a], self.pb[2 + a]
            SPb, SPbb = self.SPb[a]
            att, attb = self.att[idx % 3]
            ks = slice(kb * 128, (kb + 1) * 128)
            last = (idx == 0)
            mk.op("pe", lambda e: e.matmul(psB[:], KT[:, ks], QT[:, qs], start=True, stop=False),
                  reads=[self.ktb[kb // 4], self.qtb[qt]], writes=[pbB], skip_self=True)
            mk.op("pe", lambda e: e.matmul(psB[:], self.c_b("negtri"), SPb[:], start=False, stop=last),
                  reads=[SPbb, self.cb[1]], writes=[pbB], skip_self=True)
            if idx > 0:
                sab, sabb = self.SPaccb[(idx - 1) % 2]
                mk.op("pe", lambda e: e.matmul(psB[:], self.c_b("negones"), sab[:], start=False, stop=True),
                      reads=[sabb, self.cb[1]], writes=[pbB], skip_self=True)
            mk.op("act", lambda e: e.activation(att[:], psB[:], AF.Exp), reads=[pbB], writes=[attb])
            if i >= 0:
                m = self.c_b("mstrict%d" % i)
                mk.op("dve", lambda e: e.tensor_tensor(att[:], att[:], m, ALU.mult), reads=[attb, self.cb[1]], writes=[attb])
            if qt == 0:
                self.probe("SP%d" % idx, SPb, SPbb); self.probe("att%d" % idx, att, attb)
                if idx > 0: self.probe("sab%d" % idx, self.SPaccb[(idx - 1) % 2][0], self.SPaccb[(idx - 1) % 2][1])
            mk.op("pe", lambda e: e.matmul(psC[:], V[:, kb, :], att[:], start=(idx == 0), stop=(idx == nkb - 1)),
                  reads=[self.vtb[kb // 4], attb], writes=[pbC], skip_self=True)
            if idx < nkb - 1:
                if idx == 0:
                    mk.op("pool", lambda e: e.tensor_copy(SPacc[:], SPb[:]), reads=[SPbb], writes=[SPaccB])
                else:
                    mk.op("pool", lambda e: e.tensor_tensor(SPacc[:], SPacc[:], SPb[:], ALU.add), reads=[SPbb, SPaccB], writes=[SPaccB])
                sab2, sabb2 = self.SPaccb[idx % 2]
                mk.op("dve", lambda e: e.tensor_copy(sab2[:], SPacc[:]), reads=[SPaccB], writes=[sabb2])
        s1(0)
        for idx in range(nkb):
            if idx + 1 < nkb:
                s1(idx + 1)
            s2(idx)
        ost, ostb = self.ost[qt % 2]
        mk.op("dve", lambda e: e.tensor_copy(ost[:], psC[:]), reads=[pbC], writes=[ostb])
        mk.dma("sp", self.ysba[hs, b, :, qs], ost[:], reads=[ostb], writes=[self.Ysba], sembuf=ostb)

    def unit_diff(self, hs, b):
        mk = self.mk; NT = self.NT
        self.load_w(self.wdiff[hs], 3)
        QT, _ = self.QT; KT, _ = self.KT; V, _ = self.V
        vc, vcb = self.vc
        sm, smb = self.sm
        t0, t0b = self.f32t[0]
        mk.op("dve", lambda e: e.tensor_tensor(t0[:, 0:64], vc[:, 0:64], vc[:, 64:128], ALU.mult), reads=[vcb], writes=[t0b])
        mk.op("dve", lambda e: e.tensor_reduce(sm[:, 0:1], t0[:, 0:64], AX.X, ALU.add), reads=[t0b], writes=[smb])
        mk.op("dve", lambda e: e.tensor_tensor(t0[:, 64:128], vc[:, 128:192], vc[:, 192:256], ALU.mult), reads=[vcb], writes=[t0b])
        mk.op("dve", lambda e: e.tensor_reduce(sm[:, 1:2], t0[:, 64:128], AX.X, ALU.add), reads=[t0b], writes=[smb])
        mk.op("act", lambda e: e.activation(sm[:, 2:4], sm[:, 0:2], AF.Exp), reads=[smb], writes=[smb])
        mk.op("dve", lambda e: e.tensor_tensor(sm[:, 4:5], sm[:, 3:4], sm[:, 2:3], ALU.subtract), reads=[smb], writes=[smb])
        mk.op("dve", lambda e: e.tensor_tensor(sm[:, 5:6], sm[:, 4:5], vc[:, 256:257], ALU.subtract), reads=[smb, vcb], writes=[smb])
        mk.op("dve", lambda e: e.tensor_tensor(sm[:, 6:7], vc[:, 258:259], vc[:, 257:258], ALU.mult), reads=[smb, vcb], writes=[smb])
        def rope(src_ps, src_pb, dst, dstb, t, scl):
            qf, qfb = self.f32t[1]; A, Ab = self.f32t[2]; Bm, Bb = self.f32t[3]
            rcs, rcb = self.rc[t % 2]; rss, rsb = self.rs[t % 2]
            mk.op("act", lambda e: e.activation(qf[:], src_ps[:], AF.Copy, scale=scl), reads=[src_pb], writes=[qfb])
            mk.op("dve", lambda e: e.tensor_tensor(A[:], qf[:], rcs[:], ALU.mult), reads=[qfb, rcb], writes=[Ab])
            for (o, s_) in ((0, 32), (32, 0), (64, 96), (96, 64)):
                mk.op("pool", lambda e, o=o, s_=s_: e.tensor_tensor(Bm[o:o + 32, :], qf[s_:s_ + 32, :], rss[s_:s_ + 32, :], ALU.mult),
                      reads=[qfb, rsb], writes=[Bb])
            mk.op("dve", lambda e: e.tensor_tensor(dst[:, t * TT:(t + 1) * TT], A[:], Bm[:], ALU.add), reads=[Ab, Bb], writes=[dstb])
        def proj(t):
            xt, xb = self.load_xt(b, t)
            rcs, rcb = self.rc[t % 2]; rss, rsb = self.rs[t % 2]
            mk.dma("sp", rcs[:], self.ropec[:, t * TT:(t + 1) * TT], writes=[rcb])
            mk.dma("sp", rss[:], self.ropes[:, t * TT:(t + 1) * TT], writes=[rsb])
            self.proj_fm(xt, xb, 0, 6)
            rope(self.ps[6], self.pb[6], QT, self.qtb[t], t, 0.125)
            self.proj_fm(xt, xb, 1, 7)
            rope(self.ps[7], self.pb[7], KT, self.ktb[t], t, 1.0)
            self.proj_tm(xt, xb, 2, 6)
            mk.op("dve", lambda e: e.tensor_copy(V[:, 4 * t:4 * t + 4, :], self.ps[6][:].rearrange("p (a d) -> p a d", a=4)), reads=[self.pb[6]], writes=[self.vtb[t]])
        proj(0)
        for qt in range(NT):
            if qt + 1 < NT:
                proj(qt + 1)
            self.diff_attn(hs, b, qt)

    def diff_attn(self, hs, b, qt):
        mk = self.mk
        QT, _ = self.QT; KT, _ = self.KT; V, _ = self.V
        sm, smb = self.sm
        nkb = 4 * qt + 4
        qs = slice(qt * TT, (qt + 1) * TT)
        seq = [(kb, c) for kb in range(nkb) for c in range(2)]
        n = len(seq)
        def s1(idx):
            kb, c = seq[idx]; a = idx % 2; i = kb - 4 * qt
            psA, pbA = self.ps[a], self.pb[a]
            P, Pb = self.att[idx % 3]
            ks = slice(kb * 128, (kb + 1) * 128); cs = slice(64 * c, 64 * c + 64)
            mk.op("pe", lambda e: e.matmul(psA[:], KT[cs, ks], QT[cs, qs], start=True, stop=True),
                  reads=[self.ktb[kb // 4], self.qtb[qt]], writes=[pbA], skip_self=True)
            mk.op("act", lambda e: e.activation(P[:], psA[:], AF.Exp), reads=[pbA], writes=[Pb])
            if i >= 0:
                m = self.c_b("mincl%d" % i)
                mk.op("dve", lambda e: e.tensor_tensor(P[:], P[:], m, ALU.mult), reads=[Pb, self.cb[1]], writes=[Pb])
        def s2(idx):
            kb, c = seq[idx]
            P, Pb = self.att[idx % 3]
            psO, pbO = self.ps[2 + c], self.pb[2 + c]
            psL, pbL = self.ps[4 + c], self.pb[4 + c]
            first = (kb == 0); last = (kb == nkb - 1)
            mk.op("pe", lambda e: e.matmul(psO[:], V[:, kb, :], P[:], start=first, stop=last),
                  reads=[self.vtb[kb // 4], Pb], writes=[pbO], skip_self=True)
            mk.op("pe", lambda e: e.matmul(psL[:], self.c_b("ones"), P[:], start=first, stop=last),
                  reads=[self.cb[1], Pb], writes=[pbL], skip_self=True)
        s1(0)
        for idx in range(n):
            if idx + 1 < n:
                s1(idx + 1)
            s2(idx)
        r, rb = self.f32t[4]; o0, o0b = self.f32t[5]; o1, o1b = self.f32t[0]
        mk.op("dve", lambda e: e.reciprocal(r[:], self.ps[4][:]), reads=[self.pb[4]], writes=[rb])
        mk.op("dve", lambda e: e.tensor_tensor(o0[:], self.ps[2][:], r[:], ALU.mult), reads=[self.pb[2], rb], writes=[o0b])
        mk.op("dve", lambda e: e.reciprocal(r[:], self.ps[5][:]), reads=[self.pb[5]], writes=[rb])
        mk.op("dve", lambda e: e.tensor_tensor(o1[:], self.ps[3][:], r[:], ALU.mult), reads=[self.pb[3], rb], writes=[o1b])
        mk.op("dve", lambda e: e.scalar_tensor_tensor(o0[:], o1[:], sm[:, 5:6], o0[:], ALU.mult, ALU.add), reads=[o1b, o0b, smb], writes=[o0b])
        sq, sqb = self.Et[0]
        mk.op("act", lambda e: e.activation(sq[:], o0[:], AF.Square), reads=[o0b], writes=[sqb])
        psS, pbS = self.ps[0], self.pb[0]
        mk.op("pe", lambda e: e.matmul(psS[:], self.c_f("ones"), sq[:], start=True, stop=True), reads=[self.cf[1], sqb], writes=[pbS], skip_self=True)
        mk.op("act", lambda e: e.activation(r[:], psS[:], AF.Ln, bias=self.epst[0][:, 0:1], scale=1.0 / 128), reads=[pbS, self.epst[1]], writes=[rb])
        mk.op("act", lambda e: e.activation(r[:], r[:], AF.Exp, scale=-0.5), reads=[rb], writes=[rb])
        ost, ostb = self.ost[qt % 2]
        mk.op("dve", lambda e: e.scalar_tensor_tensor(ost[:], o0[:], sm[:, 6:7], r[:], ALU.mult, ALU.mult), reads=[o0b, rb, smb], writes=[ostb])
        mk.dma("sp", self.ydiff[hs, b, :, qs], ost[:], reads=[ostb], writes=[self.Ydiff], sembuf=ostb)

    def gdn_alloc(self):
        if hasattr(self, "g_done"): return
        self.g_done = True
        mk = self.mk
        T = self.T
        self.g_raw = [[T("graw%d_%d" % (m, p), [128, 3 + TT], F32) for p in range(2)] for m in range(3)]
        self.g_cv = [T("gcv%d" % m, [128, TT], F32) for m in range(3)]
        self.g_tmp = [T("gtmp%d" % m, [128, TT], F32) for m in range(4)]
        self.g_zs = T("gzs", [128, 4, 128], F32)
        self.g_nwz = T("gnwz", [128, 4, 128], F32)
        self.g_ktm = T("gktm", [128, 4, 128], F32)
        self.g_vtm = T("gvtm", [128, 4, 128], F32)
        self.g_ab = T("gab", [128, 8], F32)
        self.g_s = T("gs", [128, 64], F32)
        self.g_sm = T("gsm", [128, 8], F32)
        self.g_D4 = T("gD4", [128, 4, 128], F32)
        self.g_gcB = T("ggcB", [128, 4, 128], F32)
        self.Sst = T("Sst", [128, 128], F32)
        names = ["t1", "dec", "t2", "decT", "M", "N", "X0", "X1", "P0", "P1", "PT0", "PT1", "intraT", "vb", "kbg", "u", "wT", "egcB", "qdT", "kdec", "vnew", "ost", "o", "sq", "y"]
        self.g_blk = [{n: T("g_%s_%d" % (n, s), [128, 128], F32) for n in names} for s in range(4)]
        self.g_ed = T("ged", [128, 4], F32)
        self.g_cd = T("gcd", [128, 8], F32)
        self.g_r = T("gr", [128, 8], F32)
        self.qi = 0; self.wi_ = 0

    def qg(self):
        i = self.qi % 4; self.qi += 1
        bank = self.ps[4 + i]; tok = self.pb[4 + i]
        return [(bank[:, q * 128:(q + 1) * 128], tok) for q in range(4)]

    def qb(self):
        return self.qg()[0]

    def wbk(self):
        i = self.wi_ % 4; self.wi_ += 1
        return self.ps[i], self.pb[i]

    def unit_gdn(self, hs, b):
        mk = self.mk; do = mk.do
        self.gdn_alloc()
        mk.barrier()
        self.load_w(self.wgdn[hs], 4)
        wab, wabb = self.wab
        mk.dma("pool", wab[:], self.wgab, writes=[wabb])
        vc, vcb = self.vc
        gsm, gsmb = self.g_sm
        do("act", "activation", gsm[:, 0:1], vc[:, 290 + hs:291 + hs], AF.Exp, reads=[vcb], writes=[gsmb])
        do("dve", "tensor_scalar", gsm[:, 0:1], gsm[:, 0:1], -1.0, None, ALU.mult, reads=[gsmb], writes=[gsmb])
        Sst, Sstb = self.Sst
        do("dve", "memset", Sst[:], 0.0, writes=[Sstb])
        for m in range(3):
            raw, rawb = self.g_raw[m][1]
            do("pool", "memset", raw[:, TT:TT + 3], 0.0, writes=[rawb])
        for t in range(self.NT):
            self.gdn_tile(hs, b, t)

    def silu_inplace(self, x, xb, tmp, tmpb, eng2="pool"):
        do = self.mk.do
        do("act", "activation", tmp, x, AF.Exp, scale=-1.0, reads=[xb], writes=[tmpb])
        do(eng2, "tensor_scalar", tmp, tmp, 1.0, None, ALU.add, reads=[tmpb], writes=[tmpb])
        do("dve", "reciprocal", tmp, tmp, reads=[tmpb], writes=[tmpb])
        do(eng2, "tensor_tensor", x, x, tmp, ALU.mult, reads=[xb, tmpb], writes=[xb])

    def gdn_tile(self, hs, b, t):
        mk = self.mk; do = mk.do
        par = t % 2
        vc, vcb = self.vc
        cf, cfb = self.cf
        ident = self.c_f("ident"); ones = self.c_f("ones")
        xt, xb = self.load_xt(b, t)
        for m in range(3):
            ps, pb = self.wbk()
            w, wb = self.w[m]
            for k in range(32):
                do("pe", "matmul", ps[:], w[:, k, :], xt[:, k, :], start=(k == 0), stop=(k == 31), reads=[wb, xb], writes=[pb], skip_self=True)
            raw, rawb = self.g_raw[m][par]; praw, prawb = self.g_raw[m][1 - par]
            do("act" if m != 1 else "dve", "activation" if m != 1 else "tensor_copy", raw[:, 3:3 + TT], ps[:], *( [AF.Copy] if m != 1 else []), reads=[pb], writes=[rawb])
            do("pool", "tensor_copy", raw[:, 0:3], praw[:, TT:TT + 3], reads=[prawb], writes=[rawb])
        ps, pb = self.wbk()
        w, wb = self.w[3]
        for blk in range(4):
            for k in range(32):
                do("pe", "matmul", ps[:, blk * 128:(blk + 1) * 128], xt[:, k, blk * 128:(blk + 1) * 128], w[:, k, :], start=(k == 0), stop=(k == 31),
                   reads=[wb, xb], writes=[pb], skip_self=True)
        zs, zsb = self.g_zs
        do("act", "activation", zs[:].rearrange("p a d -> p (a d)"), ps[:], AF.Copy, reads=[pb], writes=[zsb])
        wab, wabb = self.wab
        pq, pqb = self.qb()
        for blk in range(4):
            for k in range(32):
                do("pe", "matmul", pq[:, blk * 2:blk * 2 + 2], xt[:, k, blk * 128:(blk + 1) * 128], wab[:, k, 2 * hs:2 * hs + 2], start=(k == 0), stop=(k == 31),
                   reads=[wabb, xb], writes=[pqb], skip_self=True)
        ab, abb = self.g_ab
        do("dve", "tensor_copy", ab[:], pq[:, 0:8], reads=[pqb], writes=[abb])
        if getattr(self, "stop_after", None) == "proj": return
        for m in range(3):
            raw, rawb = self.g_raw[m][par]
            cv, cvb = self.g_cv[m]
            eng = "dve"
            c0 = 260 + (hs * 3 + m) * 4
            do(eng, "tensor_scalar", cv[:], raw[:, 0:TT], vc[:, c0:c0 + 1], None, ALU.mult, reads=[rawb, vcb], writes=[cvb])
            for j in range(1, 4):
                do(eng, "scalar_tensor_tensor", cv[:], raw[:, j:j + TT], vc[:, c0 + j:c0 + j + 1], cv[:], ALU.mult, ALU.add, reads=[rawb, vcb, cvb], writes=[cvb])
            tmp, tmpb = self.g_tmp[m]
            self.silu_inplace(cv[:], cvb, tmp[:], tmpb)
        if getattr(self, "stop_after", None) == "conv": return
        for m in range(2):
            cv, cvb = self.g_cv[m]; tmp, tmpb = self.g_tmp[m]
            do("act", "activation", tmp[:], cv[:], AF.Square, reads=[cvb], writes=[tmpb])
            ps, pb = self.wbk()
            do("pe", "matmul", ps[:], ones, tmp[:], start=True, stop=True, reads=[cfb, tmpb], writes=[pb], skip_self=True)
            do("act", "activation", tmp[:], ps[:], AF.Ln, bias=self.epst[0][:, 1:2], reads=[pb, self.epst[1]], writes=[tmpb])
            do("act", "activation", tmp[:], tmp[:], AF.Exp, scale=-0.5, reads=[tmpb], writes=[tmpb])
            if m == 0:
                do("dve", "scalar_tensor_tensor", cv[:], cv[:], 128 ** -0.5, tmp[:], ALU.mult, ALU.mult, reads=[cvb, tmpb], writes=[cvb])
            else:
                do("dve", "tensor_tensor", cv[:], cv[:], tmp[:], ALU.mult, reads=[cvb, tmpb], writes=[cvb])
        qTn, qTnb = self.g_cv[0]; kTn, kTnb = self.g_cv[1]; vT, vTb = self.g_cv[2]
        if getattr(self, "stop_after", None) == "l2": return
        ktm, ktmb = self.g_ktm; vtm, vtmb = self.g_vtm
        for (src, srcb, dst, dstb, eng) in ((kTn, kTnb, ktm, ktmb, "act"), (vT, vTb, vtm, vtmb, "dve")):
            ps, pb = self.wbk()
            for blk in range(4):
                do("pe", "matmul", ps[:, blk * 128:(blk + 1) * 128], src[:, blk * 128:(blk + 1) * 128], ident, start=True, stop=True,
                   reads=[srcb, cfb], writes=[pb], skip_self=True)
            if eng == "act":
                do("act", "activation", dst[:].rearrange("p a d -> p (a d)"), ps[:], AF.Copy, reads=[pb], writes=[dstb])
            else:
                do("dve", "tensor_copy", dst[:].rearrange("p a d -> p (a d)"), ps[:], reads=[pb], writes=[dstb])
        if getattr(self, "stop_after", None) == "tr": return
        zs2 = zs[:].rearrange("p a d -> p (a d)")
        tmp, tmpb = self.g_tmp[3]
        self.silu_inplace(zs2, zsb, tmp[:], tmpb)
        nwz, nwzb = self.g_nwz
        for blk in range(4):
            do("pool", "tensor_tensor", nwz[:, blk, :], zs[:, blk, :], vc[:, 300:428], ALU.mult, reads=[zsb, vcb], writes=[nwzb])
        if getattr(self, "stop_after", None) == "zn": return
        gs, gsb = self.g_s
        gsm, gsmb = self.g_sm
        ab2 = ab[:].rearrange("p (k two) -> p two k", two=2)
        a4 = ab2[:, 0, :]; b4 = ab2[:, 1, :]
        do("act", "activation", gs[:, 28:32], a4, AF.Exp, bias=vc[:, 292 + hs:293 + hs], reads=[abb, vcb], writes=[gsb])
        do("act", "activation", gs[:, 28:32], gs[:, 28:32], AF.Ln, bias=1.0, reads=[gsb], writes=[gsb])
        do("dve", "tensor_scalar", gs[:, 0:4], gs[:, 28:32], gsm[:, 0:1], None, ALU.mult, reads=[gsb, gsmb], writes=[gsb])
        do("act", "activation", gs[:, 4:8], b4, AF.Exp, scale=-1.0, reads=[abb], writes=[gsb])
        do("dve", "tensor_scalar", gs[:, 4:8], gs[:, 4:8], 1.0, None, ALU.add, reads=[gsb], writes=[gsb])
        do("dve", "reciprocal", gs[:, 4:8], gs[:, 4:8], reads=[gsb], writes=[gsb])
        pq, pqb = self.qb()
        do("pe", "matmul", pq[:, 0:4], self.c_f("bt"), gs[:, 0:4], start=True, stop=True, reads=[cfb, gsb], writes=[pqb], skip_self=True)
        do("dve", "tensor_copy", gs[:, 8:12], pq[:, 0:4], reads=[pqb], writes=[gsb])
        do("dve", "tensor_scalar", gs[:, 12:16], gs[:, 8:12], -1.0, None, ALU.mult, reads=[gsb], writes=[gsb])
        do("act", "activation", gs[:, 16:20], gs[:, 8:12], AF.Exp, reads=[gsb], writes=[gsb])
        do("dve", "tensor_tensor", gs[:, 20:24], gs[:, 4:8], gs[:, 16:20], ALU.mult, reads=[gsb], writes=[gsb])
        do("dve", "tensor_scalar", gs[:, 24:28], gs[:, 4:8], -1.0, None, ALU.mult, reads=[gsb], writes=[gsb])
        if getattr(self, "stop_after", None) == "gb": return
        D4, D4b = self.g_D4; gcB, gcBb = self.g_gcB
        for blk in range(4):
            do("dve", "tensor_scalar", D4[:, blk, :], ident, gs[:, 8 + blk:9 + blk], None, ALU.mult, reads=[cfb, gsb], writes=[D4b])
        ps, pb = self.wbk()
        do("pe", "matmul", ps[:], ones, D4[:].rearrange("p a d -> p (a d)"), start=True, stop=True, reads=[cfb, D4b], writes=[pb], skip_self=True)
        do("act", "activation", gcB[:].rearrange("p a d -> p (a d)"), ps[:], AF.Copy, reads=[pb], writes=[gcBb])
        ed, edb = self.g_ed; cd, cdb = self.g_cd
        if getattr(self, "stop_after", None) == "gcb": return
        B_ = self.g_blk
        R = range(4)
        bs = [slice(blk * 128, (blk + 1) * 128) for blk in R]
        nms = self.c_f("negm_strict"); nmi = self.c_f("negm_inclT")
        for blk in R:
            d = B_[blk]
            do("dve", "scalar_tensor_tensor", d["t1"][0][:], gcB[:, blk, :], -1.0, nms, ALU.mult, ALU.add, reads=[gcBb, cfb], writes=[d["t1"][1]])
            do("act", "activation", d["dec"][0][:], d["t1"][0][:], AF.Exp, bias=gs[:, 8 + blk:9 + blk], reads=[d["t1"][1], gsb], writes=[d["dec"][1]])
            do("pool", "tensor_tensor", d["t2"][0][:], gcB[:, blk, :], nmi, ALU.add, reads=[gcBb, cfb], writes=[d["t2"][1]])
            do("act", "activation", d["decT"][0][:], d["t2"][0][:], AF.Exp, bias=gs[:, 12 + blk:13 + blk], reads=[d["t2"][1], gsb], writes=[d["decT"][1]])
            do("act", "activation", d["egcB"][0][:], gcB[:, blk, :], AF.Exp, reads=[gcBb], writes=[d["egcB"][1]])
            do("pool", "tensor_tensor", d["qdT"][0][:], qTn[:, bs[blk]], d["egcB"][0][:], ALU.mult, reads=[qTnb, d["egcB"][1]], writes=[d["qdT"][1]])
            for c in range(2):
                r = slice(64 * c, 64 * c + 64)
                do("act", "activation", ed[r, blk:blk + 1], gcB[r, blk, 64 * c + 63:64 * c + 64], AF.Exp, bias=gs[r, 12 + blk:13 + blk], reads=[gcBb, gsb], writes=[edb])
                do("act", "activation", cd[:, 2 * blk + c:2 * blk + c + 1], gcB[:, blk, 64 * c + 63:64 * c + 64], AF.Exp, reads=[gcBb], writes=[cdb])
            do("dve", "tensor_scalar", d["kdec"][0][:], ktm[:, blk, :], ed[:, blk:blk + 1], None, ALU.mult, reads=[ktmb, edb], writes=[d["kdec"][1]])
            do("dve", "tensor_scalar", d["vb"][0][:], vtm[:, blk, :], gs[:, 4 + blk:5 + blk], None, ALU.mult, reads=[vtmb, gsb], writes=[d["vb"][1]])
            do("dve", "tensor_scalar", d["kbg"][0][:], ktm[:, blk, :], gs[:, 20 + blk:21 + blk], None, ALU.mult, reads=[ktmb, gsb], writes=[d["kbg"][1]])
        if getattr(self, "stop_after", None) == "prep0": return
        pG = self.qg()
        for blk in R:
            do("pe", "matmul", pG[blk][0], kTn[:, bs[blk]], kTn[:, bs[blk]], start=True, stop=True, reads=[kTnb], writes=[pG[blk][1]], skip_self=True)
        for blk in R:
            d = B_[blk]
            do("dve", "scalar_tensor_tensor", d["M"][0][:], pG[blk][0], gs[:, 24 + blk:25 + blk], d["dec"][0][:], ALU.mult, ALU.mult,
               reads=[pG[blk][1], gsb, d["dec"][1]], writes=[d["M"][1]])
        if t == 0:
            self.probe("M0", B_[0]["M"][0], B_[0]["M"][1]); self.probe("dec0", B_[0]["dec"][0], B_[0]["dec"][1]); self.probe("gs", gs, gsb)
            self.probe("kTn", kTn, kTnb); self.probe("gcB", gcB, gcBb); self.probe("ktm", ktm, ktmb)
        import os
        if os.environ.get("GDN_DUMMY") == "1":
            for blk in R:
                d = B_[blk]
                do("dve", "tensor_copy", d["sq"][0][:], d["M"][0][:], reads=[d["M"][1]], writes=[d["sq"][1], d["M"][1]])
        if getattr(self, "stop_after", None) == "pg": return
        pN = self.qg()
        for blk in R:
            d = B_[blk]
            import os
            if os.environ.get("GDN_LHS") == "dec":
                do("pe", "matmul", pN[blk][0], d["dec"][0][:], ident, start=True, stop=True, reads=[d["dec"][1], cfb], writes=[pN[blk][1]], skip_self=True)
            else:
                do("pe", "matmul", pN[blk][0], d["M"][0][:], ident, start=True, stop=True, reads=[d["M"][1], cfb], writes=[pN[blk][1]], skip_self=True)
        import os
        if os.environ.get("GDN_VAR") == "1": return
        for blk in R:
            d = B_[blk]
            do("act", "activation", d["N"][0][:], pN[blk][0], AF.Copy, reads=[pN[blk][1]], writes=[d["N"][1]])
            if os.environ.get("GDN_VAR") == "2": continue
            do("dve", "tensor_tensor", d["X0"][0][:], d["N"][0][:], ident, ALU.add, reads=[d["N"][1], cfb], writes=[d["X0"][1]])
        if getattr(self, "stop_after", None) == "pn": return
        pQ = self.qg()
        for blk in R:
            do("pe", "matmul", pQ[blk][0], kTn[:, bs[blk]], qTn[:, bs[blk]], start=True, stop=True, reads=[kTnb, qTnb], writes=[pQ[blk][1]], skip_self=True)
        for blk in R:
            d = B_[blk]
            do("dve", "tensor_tensor", d["intraT"][0][:], pQ[blk][0], d["decT"][0][:], ALU.mult, reads=[pQ[blk][1], d["decT"][1]], writes=[d["intraT"][1]])
        if getattr(self, "stop_after", None) == "prep1": return
        cur = [{"P": B_[blk]["N"], "PT": B_[blk]["M"], "X": B_[blk]["X0"]} for blk in R]
        for lvl in range(1, 6):
            pp = lvl % 2
            pa = self.qg()
            for blk in R:
                c_ = cur[blk]
                do("pe", "matmul", pa[blk][0], c_["P"][0][:], c_["PT"][0][:], start=True, stop=True, reads=[c_["P"][1], c_["PT"][1]], writes=[pa[blk][1]], skip_self=True)
            pb_ = None
            if lvl < 5:
                pb_ = self.qg()
                for blk in R:
                    c_ = cur[blk]
                    do("pe", "matmul", pb_[blk][0], c_["PT"][0][:], c_["P"][0][:], start=True, stop=True, reads=[c_["P"][1], c_["PT"][1]], writes=[pb_[blk][1]], skip_self=True)
            for blk in R:
                d = B_[blk]
                nPT = d["PT%d" % pp]
                do("act", "activation", nPT[0][:], pa[blk][0], AF.Copy, reads=[pa[blk][1]], writes=[nPT[1]])
                if lvl < 5:
                    nP = d["P%d" % pp]
                    do("dve", "tensor_copy", nP[0][:], pb_[blk][0], reads=[pb_[blk][1]], writes=[nP[1]])
                    cur[blk]["P"] = nP
                cur[blk]["PT"] = nPT
            px = self.qg()
            for blk in R:
                c_ = cur[blk]
                do("pe", "matmul", px[blk][0], c_["PT"][0][:], c_["X"][0][:], start=True, stop=True, reads=[c_["PT"][1], c_["X"][1]], writes=[px[blk][1]], skip_self=True)
            for blk in R:
                d = B_[blk]
                nX = d["X%d" % pp]
                do("dve", "tensor_tensor", nX[0][:], px[blk][0], cur[blk]["X"][0][:], ALU.add, reads=[px[blk][1], cur[blk]["X"][1]], writes=[nX[1]])
                cur[blk]["X"] = nX
        if getattr(self, "stop_after", None) == "neumann": return
        pu = self.qg()
        for blk in R:
            d = B_[blk]; X = cur[blk]["X"]
            do("pe", "matmul", pu[blk][0], X[0][:], d["vb"][0][:], start=True, stop=True, reads=[X[1], d["vb"][1]], writes=[pu[blk][1]], skip_self=True)
        for blk in R:
            d = B_[blk]
            do("act", "activation", d["u"][0][:], pu[blk][0], AF.Copy, reads=[pu[blk][1]], writes=[d["u"][1]])
        pw = self.qg()
        for blk in R:
            d = B_[blk]; X = cur[blk]["X"]
            do("pe", "matmul", pw[blk][0], d["kbg"][0][:], X[0][:], start=True, stop=True, reads=[X[1], d["kbg"][1]], writes=[pw[blk][1]], skip_self=True)
        for blk in R:
            d = B_[blk]
            do("dve", "tensor_copy", d["wT"][0][:], pw[blk][0], reads=[pw[blk][1]], writes=[d["wT"][1]])
        if getattr(self, "stop_after", None) == "uw": return
        Sst, Sstb = self.Sst
        gr, grb = self.g_r
        for blk in R:
            d = B_[blk]
            for c in range(2):
                r = slice(64 * c, 64 * c + 64)
                pA, pAb = self.qb(); pO, pOb = self.qb(); pU, pUb = self.qb()
                do("pe", "matmul", pA, d["wT"][0][:], Sst[:], start=True, stop=True, reads=[d["wT"][1], Sstb], writes=[pAb], skip_self=True)
                do("pe", "matmul", pO, d["qdT"][0][:], Sst[:], start=True, stop=True, reads=[d["qdT"][1], Sstb], writes=[pOb], skip_self=True)
                do("dve", "tensor_tensor", d["vnew"][0][r, :], d["u"][0][r, :], pA[r, :], ALU.subtract, reads=[d["u"][1], pAb], writes=[d["vnew"][1]])
                do("act", "activation", d["ost"][0][r, :], pO[r, :], AF.Copy, reads=[pOb], writes=[d["ost"][1]])
                do("pe", "matmul", pU, d["kdec"][0][r, :], d["vnew"][0][r, :], start=True, stop=True, reads=[d["kdec"][1], d["vnew"][1]], writes=[pUb], skip_self=True)
                do("dve", "scalar_tensor_tensor", Sst[:], Sst[:], cd[:, 2 * blk + c:2 * blk + c + 1], pU, ALU.mult, ALU.add, reads=[Sstb, cdb, pUb], writes=[Sstb])
            pI, pIb = self.qb()
            do("pe", "matmul", pI, d["intraT"][0][:], d["vnew"][0][:], start=True, stop=True, reads=[d["intraT"][1], d["vnew"][1]], writes=[pIb], skip_self=True)
            do("dve", "tensor_tensor", d["o"][0][:], d["ost"][0][:], pI, ALU.add, reads=[d["ost"][1], pIb], writes=[d["o"][1]])
            do("pool", "tensor_tensor", d["sq"][0][:], d["o"][0][:], d["o"][0][:], ALU.mult, reads=[d["o"][1]], writes=[d["sq"][1]])
            do("dve", "tensor_reduce", gr[:, blk:blk + 1], d["sq"][0][:], AX.X, ALU.add, reads=[d["sq"][1]], writes=[grb])
            do("act", "activation", gr[:, 4 + blk:5 + blk], gr[:, blk:blk + 1], AF.Ln, bias=self.epst[0][:, 1:2], scale=1.0 / 128, reads=[grb, self.epst[1]], writes=[grb])
            do("act", "activation", gr[:, 4 + blk:5 + blk], gr[:, 4 + blk:5 + blk], AF.Exp, scale=-0.5, reads=[grb], writes=[grb])
            do("dve", "scalar_tensor_tensor", d["y"][0][:], d["o"][0][:], gr[:, 4 + blk:5 + blk], nwz[:, blk, :], ALU.mult, ALU.mult, reads=[d["o"][1], grb, nwzb], writes=[d["y"][1]])
            t0 = t * TT + blk * 128
            mk.dma("sp", self.ygdn[hs, b, t0:t0 + 128, :], d["y"][0][:], reads=[d["y"][1]], writes=[self.Ygdn], sembuf=d["y"][1])

ALPHA = (2 * 2) ** 0.25

class L2:
    def __init__(self, TC, TT2):
        self.TC, self.TT2 = TC, TT2
        self.NTL = TC // TT2

    def build(self):
        TC, TT2 = self.TC, self.TT2
        nc = bass.Bass("TRN2", target_bir_lowering=False); self.nc = nc
        D = lambda n, s, kind="ExternalInput": nc.dram_tensor(n, s, F32, kind=kind).ap()
        xT = D("xT", [128, 32, TC]); yT = D("yT", [3, 128, 16, TC])
        wgate = D("wgate", [3, 32, 128, 32, 128])
        wbr = D("wbr", [3, 32, 128, 16, 128])
        wout = D("wout", [32, 128, 32, 128])
        lnv = D("lnv", [128, 64])
        ones_d = D("ones", [128, 128])
        x1T = D("x1T", [128, 32, TC], "ExternalOutput")
        with ExitStack() as st:
            mk = MK(nc, st); do = mk.do
            def T(name, shape, dt=F32):
                return st.enter_context(nc.sbuf_tensor(name, shape, dt)), mk.buf(name)
            ps = [st.enter_context(nc.psum_tensor("ps%d" % i, [128, 512], F32)) for i in range(8)]
            pb = [mk.buf("ps%d" % i) for i in range(8)]
            xb_, xbb = T("xb", [128, 32, TT2], BF16)
            yb_ = [T("yb%d" % m, [128, 16, TT2], BF16) for m in range(3)]
            mg, mgb = T("mg", [128, 32, TT2], BF16)
            rT, rTb = T("rT", [128, 32, TT2], F32)
            wg_ = [[T("wg%d_%d" % (m, s), [128, 32, 128], BF16) for s in range(2)] for m in range(3)]
            wb_ = [[T("wb%d_%d" % (m, s), [128, 16, 128], BF16) for s in range(2)] for m in range(3)]
            wo_ = [T("wo%d" % s, [128, 32, 128], BF16) for s in range(2)]
            sg = [T("sg%d" % i, [128, TT2], F32) for i in range(3)]
            acc, accb = T("acc", [128, TT2], F32)
            xr = [T("xr%d" % i, [128, TT2], F32) for i in range(2)]
            sq = [T("sq%d" % i, [128, TT2], F32) for i in range(2)]
            mean, meanb = T("mean", [128, TT2], F32); rstd, rstdb = T("rstd", [128, TT2], F32)
            tmp = [T("tmp%d" % i, [128, TT2], F32) for i in range(2)]
            ot = [T("ot%d" % i, [128, TT2], F32) for i in range(2)]
            lv, lvb = T("lv", [128, 64], F32); on, onb = T("on", [128, 128], F32)
            eps, epsb = T("eps", [128, 1], F32)
            OUT = mk.buf("OUT")
            mk.dma("sp", lv[:], lnv, writes=[lvb]); mk.dma("sp", on[:], ones_d, writes=[onb])
            do("dve", "memset", eps[:], LN_EPS, writes=[epsb])
            wcnt = 0; ocnt = 0
            for tl in range(self.NTL):
                ts = slice(tl * TT2, (tl + 1) * TT2)
                mk.dma("pool", xb_[:], xT[:, :, ts], writes=[xbb])
                for m in range(3):
                    mk.dma("pool", yb_[m][0][:], yT[m, :, :, ts], writes=[yb_[m][1]])
                for n in range(32):
                    s = wcnt % 2; wcnt += 1
                    for m in range(3):
                        mk.dma("pool", wg_[m][s][0][:], wgate[m, n], writes=[wg_[m][s][1]])
                        mk.dma("pool", wb_[m][s][0][:], wbr[m, n], writes=[wb_[m][s][1]])
                    for m in range(3):
                        pg, pgb = ps[m], pb[m]; pr, prb = ps[3 + m], pb[3 + m]
                        w, wb2 = wg_[m][s]
                        for k in range(32):
                            do("pe", "matmul", pg[:, 0:TT2], w[:, k, :], xb_[:, k, :], start=(k == 0), stop=(k == 31), reads=[wb2, xbb], writes=[pgb], skip_self=True)
                        w, wb2 = wb_[m][s]
                        for k in range(16):
                            do("pe", "matmul", pr[:, 0:TT2], w[:, k, :], yb_[m][0][:, k, :], start=(k == 0), stop=(k == 15), reads=[wb2, yb_[m][1]], writes=[prb], skip_self=True)
                        g, gb = sg[m]
                        do("act", "activation", g[:], pg[:, 0:TT2], AF.Exp, scale=-1.0, reads=[pgb], writes=[gb])
                        do("pool", "tensor_scalar", g[:], g[:], 1.0, None, ALU.add, reads=[gb], writes=[gb])
                        do("dve", "reciprocal", g[:], g[:], reads=[gb], writes=[gb])
                        if m == 0:
                            do("dve", "tensor_tensor", acc[:], pr[:, 0:TT2], g[:], ALU.mult, reads=[prb, gb], writes=[accb])
                        else:
                            do("dve", "tensor_tensor", g[:], pr[:, 0:TT2], g[:], ALU.mult, reads=[prb, gb], writes=[gb])
                            if m == 1:
                                do("pool", "tensor_tensor", acc[:], acc[:], g[:], ALU.add, reads=[accb, gb], writes=[accb])
                            else:
                                do("pool", "tensor_tensor", mg[:, n, :], acc[:], g[:], ALU.add, reads=[accb, gb], writes=[mgb])
                for e in range(32):
                    s = wcnt % 2; wcnt += 1
                    w, wb2 = wo_[s]
                    mk.dma("pool", w[:], wout[e], writes=[wb2])
                    x_, x_b = xr[e % 2]
                    mk.dma("sp", x_[:], xT[:, e, ts], writes=[x_b])
                    ph, phb = ps[6 + e % 2], pb[6 + e % 2]
                    for n in range(32):
                        do("pe", "matmul", ph[:, 0:TT2], w[:, n, :], mg[:, n, :], start=(n == 0), stop=(n == 31), reads=[wb2, mgb], writes=[phb], skip_self=True)
                    do("dve", "scalar_tensor_tensor", rT[:, e, :], x_[:], ALPHA, ph[:, 0:TT2], ALU.mult, ALU.add, reads=[x_b, phb], writes=[rTb])
                self.layer_norm(mk, rT, rTb, ps, pb, on, onb, sq, mean, meanb, rstd, rstdb, tmp, eps, epsb, lv, lvb, ot, x1T, ts, OUT, TT2)
            mk.finish([OUT])
        return nc

    @staticmethod
    def layer_norm(mk, rT, rTb, ps, pb, on, onb, sq, mean, meanb, rstd, rstdb, tmp, eps, epsb, lv, lvb, ot, outT, ts, OUT, TT2):
        do = mk.do
        p1, p1b = ps[0], pb[0]; p2, p2b = ps[1], pb[1]
        for e in range(32):
            do("pe", "matmul", p1[:, 0:TT2], on[:], rT[:, e, :], start=(e == 0), stop=(e == 31), reads=[onb, rTb], writes=[p1b], skip_self=True)
        for e in range(32):
            q, qb = sq[e % 2]
            do("act", "activation", q[:], rT[:, e, :], AF.Square, reads=[rTb], writes=[qb])
            do("pe", "matmul", p2[:, 0:TT2], on[:], q[:], start=(e == 0), stop=(e == 31), reads=[onb, qb], writes=[p2b], skip_self=True)
        t0, t0b = tmp[0]
        do("dve", "tensor_scalar", mean[:], p1[:, 0:TT2], 1.0 / 4096, None, ALU.mult, reads=[p1b], writes=[meanb])
        do("dve", "tensor_tensor", t0[:], mean[:], mean[:], ALU.mult, reads=[meanb], writes=[t0b])
        do("dve", "scalar_tensor_tensor", t0[:], p2[:, 0:TT2], 1.0 / 4096, t0[:], ALU.mult, ALU.subtract, reads=[p2b, t0b], writes=[t0b])
        do("act", "activation", rstd[:], t0[:], AF.Ln, bias=eps[:, 0:1], reads=[t0b, epsb], writes=[rstdb])
        do("act", "activation", rstd[:], rstd[:], AF.Exp, scale=-0.5, reads=[rstdb], writes=[rstdb])
        for e in range(32):
            t1, t1b = tmp[1] if e % 2 else tmp[0]
            o, ob = ot[e % 2]
            do("pool", "tensor_tensor", t1[:], rT[:, e, :], mean[:], ALU.subtract, reads=[rTb, meanb], writes=[t1b])
            do("pool", "tensor_tensor", t1[:], t1[:], rstd[:], ALU.mult, reads=[t1b, rstdb], writes=[t1b])
            do("dve", "tensor_scalar", o[:], t1[:], lv[:, e:e + 1], lv[:, 32 + e:33 + e], ALU.mult, ALU.add, reads=[t1b, lvb], writes=[ob])
            mk.dma("sp", outT[:, e, ts], o[:], reads=[ob], writes=[OUT], sembuf=ob)


def make_sel():
    sel = np.zeros((32, 32 * 128), np.float32)
    for e in range(32):
        sel[e, e * 128:(e + 1) * 128] = 1.0
    return sel

class L3:
    def __init__(self, TC, TT2):
        self.TC, self.TT2 = TC, TT2
        self.NTL = TC // TT2

    def build(self):
        TC, TT2 = self.TC, self.TT2
        nb = TT2 // 128
        nc = bass.Bass("TRN2", target_bir_lowering=False); self.nc = nc
        D = lambda n, s, kind="ExternalInput": nc.dram_tensor(n, s, F32, kind=kind).ap()
        x1T = D("x1T", [128, 32, TC])
        wr_d = D("wr", [128, 32, 36]); rb_d = D("rb", [128, 36]); sel_d = D("sel", [32, 32 * 128])
        ident_d = D("ident", [128, 128]); ones_d = D("ones", [128, 128])
        wg_d = D("wg", [32, 4, 128, 32, 128]); wu_d = D("wu", [32, 4, 128, 32, 128]); wd_d = D("wd", [32, 128, 4, 4096])
        lnv = D("lnv", [128, 64])
        x2T = D("x2T", [128, 32, TC], "ExternalOutput")
        with ExitStack() as st:
            mk = MK(nc, st); do = mk.do
            def T(name, shape, dt=F32):
                return st.enter_context(nc.sbuf_tensor("s_" + name, shape, dt)), mk.buf(name)
            ps = [st.enter_context(nc.psum_tensor("ps%d" % i, [128, 512], F32)) for i in range(8)]
            pb = [mk.buf("ps%d" % i) for i in range(8)]
            xf, xfb = T("xf", [128, 32, TT2], F32); xb_, xbb = T("xb", [128, 32, TT2], BF16)
            acc, accb = T("acc", [128, 32, TT2], F32)
            wg_ = [T("wg%d" % s, [128, 32, 128], BF16) for s in range(2)]
            wu_ = [T("wu%d" % s, [128, 32, 128], BF16) for s in range(2)]
            wd_ = [T("wd%d" % s, [128, 4, 2048], BF16) for s in range(2)]
            hT, hTb = T("hT", [128, 4, TT2], BF16)
            wr, wrb = T("wr", [128, 32, 36], F32); rb, rbb = T("rb", [128, 36], F32)
            sel, selb = T("sel", [32, 32 * 128], F32)
            ident, identb = T("ident", [128, 128], F32); on, onb = T("on", [128, 128], F32)
            lv, lvb = T("lv", [128, 64], F32); eps, epsb = T("eps", [128, 1], F32)
            lg, lgb = T("lg", [128, 36], F32)
            sm, smb = T("sm", [128, 96], F32)
            comb, combb = T("comb", [128, 32], F32)
            cT, cTb = T("cT", [32, TT2], F32)
            cB, cBb = T("cB", [128, TT2], F32)
            e1 = [T("e1_%d" % i, [128, TT2], F32) for i in range(2)]
            sq = [T("sq%d" % i, [128, TT2], F32) for i in range(2)]
            mean, meanb = T("mean", [128, TT2], F32); rstd, rstdb = T("rstd", [128, TT2], F32)
            tmp = [T("tmp%d" % i, [128, TT2], F32) for i in range(2)]
            ot = [T("ot%d" % i, [128, TT2], F32) for i in range(2)]
            OUT = mk.buf("OUT")
            for (t_, tb_, d_) in ((wr, wrb, wr_d), (rb, rbb, rb_d), (sel, selb, sel_d), (ident, identb, ident_d), (on, onb, ones_d), (lv, lvb, lnv)):
                mk.dma("sp", t_[:], d_, writes=[tb_])
            do("dve", "memset", eps[:], LN_EPS, writes=[epsb])
            wc = 0; dc = 0
            for tl in range(self.NTL):
                ts = slice(tl * TT2, (tl + 1) * TT2)
                mk.dma("sp", xf[:], x1T[:, :, ts], writes=[xfb])
                mk.dma("pool", xb_[:], x1T[:, :, ts], writes=[xbb])
                for blk in range(nb):
                    bsl = slice(blk * 128, (blk + 1) * 128)
                    p0, p0b = ps[0], pb[0]
                    for k in range(32):
                        do("pe", "matmul", p0[:, 0:36], xf[:, k, bsl], wr[:, k, :], start=(k == 0), stop=(k == 31), reads=[xfb, wrb], writes=[p0b], skip_self=True)
                    do("dve", "tensor_tensor", lg[:], p0[:, 0:36], rb[:], ALU.add, reads=[p0b, rbb], writes=[lgb])
                    S_ = lambda a, b_: sm[:, a:b_]
                    R1 = dict(reads=[smb, lgb], writes=[smb])
                    do("dve", "tensor_reduce", S_(0, 1), lg[:, 0:4], AX.X, ALU.max, **R1)
                    do("dve", "tensor_scalar", S_(1, 2), S_(0, 1), -1.0, None, ALU.mult, **R1)
                    do("dve", "tensor_scalar", S_(2, 6), lg[:, 0:4], S_(0, 1), None, ALU.is_ge, **R1)
                    do("act", "activation", S_(6, 10), lg[:, 0:4], AF.Exp, bias=S_(1, 2), **R1)
                    do("dve", "tensor_reduce", S_(10, 11), S_(6, 10), AX.X, ALU.add, **R1)
                    do("dve", "reciprocal", S_(11, 12), S_(10, 11), **R1)
                    do("dve", "tensor_scalar", S_(12, 20), lg[:, 4:12], S_(2, 3), None, ALU.mult, **R1)
                    for g in range(1, 4):
                        do("dve", "scalar_tensor_tensor", S_(12, 20), lg[:, 4 + 8 * g:12 + 8 * g], S_(2 + g, 3 + g), S_(12, 20), ALU.mult, ALU.add, **R1)
                    do("dve", "tensor_reduce", S_(20, 21), S_(12, 20), AX.X, ALU.max, **R1)
                    do("dve", "tensor_scalar", S_(21, 29), S_(12, 20), S_(20, 21), None, ALU.is_ge, **R1)
                    do("dve", "scalar_tensor_tensor", S_(29, 37), S_(21, 29), -1e30, S_(12, 20), ALU.mult, ALU.add, **R1)
                    do("dve", "tensor_reduce", S_(37, 38), S_(29, 37), AX.X, ALU.max, **R1)
                    do("dve", "tensor_scalar", S_(38, 46), S_(29, 37), S_(37, 38), None, ALU.is_ge, **R1)
                    do("dve", "tensor_tensor", S_(46, 47), S_(37, 38), S_(20, 21), ALU.subtract, **R1)
                    do("act", "activation", S_(47, 48), S_(46, 47), AF.Exp, **R1)
                    do("dve", "tensor_scalar", S_(48, 49), S_(47, 48), 1.0, None, ALU.add, **R1)
                    do("dve", "reciprocal", S_(49, 50), S_(48, 49), **R1)
                    do("dve", "tensor_tensor", S_(50, 51), S_(49, 50), S_(11, 12), ALU.mult, **R1)
                    do("dve", "tensor_tensor", S_(51, 52), S_(50, 51), S_(47, 48), ALU.mult, **R1)
                    do("dve", "tensor_scalar", S_(52, 60), S_(21, 29), S_(50, 51), None, ALU.mult, **R1)
                    do("dve", "scalar_tensor_tensor", S_(52, 60), S_(38, 46), S_(51, 52), S_(52, 60), ALU.mult, ALU.add, **R1)
                    for g in range(4):
                        do("dve", "tensor_scalar", comb[:, 8 * g:8 * g + 8], S_(52, 60), S_(2 + g, 3 + g), None, ALU.mult, reads=[smb], writes=[combb])
                    p1, p1b = ps[1], pb[1]
                    do("pe", "matmul", p1[0:32, 0:128], comb[:], ident[:], start=True, stop=True, reads=[combb, identb], writes=[p1b], skip_self=True)
                    do("act", "activation", cT[:, bsl], p1[0:32, 0:128], AF.Copy, reads=[p1b], writes=[cTb])
                for e in range(32):
                    p1, p1b = ps[1], pb[1]
                    do("pe", "matmul", p1[:, 0:TT2], sel[:, e * 128:(e + 1) * 128], cT[:], start=True, stop=True, reads=[selb, cTb], writes=[p1b], skip_self=True)
                    do("act", "activation", cB[:], p1[:, 0:TT2], AF.Copy, reads=[p1b], writes=[cBb])
                    for f in range(4):
                        s = wc % 2; wc += 1
                        wg, wgb = wg_[s]; wu, wub = wu_[s]
                        mk.dma("pool", wg[:], wg_d[e, f], writes=[wgb])
                        mk.dma("pool", wu[:], wu_d[e, f], writes=[wub])
                        pg, pgb = ps[2 + f % 2], pb[2 + f % 2]; pu, pub = ps[4 + f % 2], pb[4 + f % 2]
                        for k in range(32):
                            do("pe", "matmul", pg[:, 0:TT2], wg[:, k, :], xb_[:, k, :], start=(k == 0), stop=(k == 31), reads=[wgb, xbb], writes=[pgb], skip_self=True)
                        for k in range(32):
                            do("pe", "matmul", pu[:, 0:TT2], wu[:, k, :], xb_[:, k, :], start=(k == 0), stop=(k == 31), reads=[wub, xbb], writes=[pub], skip_self=True)
                        t1, t1b = e1[f % 2]
                        do("act", "activation", t1[:], pg[:, 0:TT2], AF.Exp, scale=-1.0, reads=[pgb], writes=[t1b])
                        do("pool", "tensor_scalar", t1[:], t1[:], 1.0, None, ALU.add, reads=[t1b], writes=[t1b])
                        do("dve", "reciprocal", t1[:], t1[:], reads=[t1b], writes=[t1b])
                        do("dve", "tensor_tensor", t1[:], pg[:, 0:TT2], t1[:], ALU.mult, reads=[pgb, t1b], writes=[t1b])
                        do("dve", "tensor_tensor", t1[:], pu[:, 0:TT2], t1[:], ALU.mult, reads=[pub, t1b], writes=[t1b])
                        do("pool", "tensor_tensor", hT[:, f, :], t1[:], cB[:], ALU.mult, reads=[t1b, cBb], writes=[hTb])
                    for half in range(2):
                        s = dc % 2; dc += 1
                        wd, wdb = wd_[s]
                        mk.dma("pool", wd[:], wd_d[e, :, :, half * 2048:(half + 1) * 2048], writes=[wdb])
                        for cc in range(16):
                            c = half * 16 + cc
                            po, pob = ps[6 + c % 2], pb[6 + c % 2]
                            for f in range(4):
                                do("pe", "matmul", po[:, 0:TT2], wd[:, f, cc * 128:(cc + 1) * 128], hT[:, f, :], start=(f == 0), stop=(f == 3), reads=[wdb, hTb], writes=[pob], skip_self=True)
                            if e == 0:
                                do("dve", "tensor_copy", acc[:, c, :], po[:, 0:TT2], reads=[pob], writes=[accb])
                            else:
                                do("dve", "tensor_tensor", acc[:, c, :], acc[:, c, :], po[:, 0:TT2], ALU.add, reads=[pob, accb], writes=[accb])
                for c in range(32):
                    do("dve", "scalar_tensor_tensor", acc[:, c, :], xf[:, c, :], ALPHA, acc[:, c, :], ALU.mult, ALU.add, reads=[xfb, accb], writes=[accb])
                L2.layer_norm(mk, acc, accb, ps, pb, on, onb, sq, mean, meanb, rstd, rstdb, tmp, eps, epsb, lv, lvb, ot, x2T, ts, OUT, TT2)
            mk.finish([OUT])
        return nc


_PROGS = {}

def _prog(key, fn):
    if key not in _PROGS:
        _PROGS[key] = fn()
    return _PROGS[key]

def _wtile(W):
    n = W.shape[1]
    return np.ascontiguousarray(np.asarray(W).reshape(32, 128, n).transpose(1, 0, 2))

def _wt(W, kchunks, nchunks):
    return np.ascontiguousarray(np.asarray(W).reshape(kchunks, 128, nchunks, 128).transpose(2, 1, 0, 3))

def _l1_inputs(inp, l, core, L, xT_all, S):
    w_in = inp["w_in"][l]
    m = {"xT": xT_all}
    hh = [2 * core, 2 * core + 1]
    kinds = L.kinds
    if "sba" in kinds:
        m["wsba"] = np.stack([np.stack([_wtile(w_in[:, o + h * 128: o + (h + 1) * 128]) for o in (0, 2048, 4096)]) for h in hh])
    if "diff" in kinds:
        m["wdiff"] = np.stack([np.stack([_wtile(w_in[:, 14368 + o + h * 128: 14368 + o + (h + 1) * 128]) for o in (0, 2048, 4096)]) for h in hh])
        cosT, sinS = make_rope(S)
        m["ropec"] = cosT; m["ropes"] = sinS
    if "gdn" in kinds:
        m["wgdn"] = np.stack([np.stack([_wtile(w_in[:, o + h * 128: o + (h + 1) * 128]) for o in (6144, 8192, 10240, 12288)]) for h in hh])
        ab = np.stack([w_in[:, 14336 + hh[0]], w_in[:, 14352 + hh[0]], w_in[:, 14336 + hh[1]], w_in[:, 14352 + hh[1]]], axis=1)
        m["wgab"] = _wtile(ab)
    vec = np.zeros((128, 1024), np.float32)
    vec[:, 0:64] = inp["diff_lambda_q1"][l][None, :]; vec[:, 64:128] = inp["diff_lambda_k1"][l][None, :]
    vec[:, 128:192] = inp["diff_lambda_q2"][l][None, :]; vec[:, 192:256] = inp["diff_lambda_k2"][l][None, :]
    lam_init = 0.8 - 0.6 * math.exp(-0.3 * l)
    vec[:, 256] = lam_init; vec[:, 257] = 1.0 - lam_init
    vec[:, 258] = inp["diff_subln_w"][l]
    cw = inp["conv_w"][l]
    for hs, h in enumerate(hh):
        for mi, o in enumerate((0, 2048, 4096)):
            vec[:, 260 + (hs * 3 + mi) * 4: 260 + (hs * 3 + mi) * 4 + 4] = cw[:, o + h * 128: o + (h + 1) * 128].T
        vec[:, 290 + hs] = inp["gdn_a_log"][l][h]; vec[:, 292 + hs] = inp["gdn_dt_bias"][l][h]
    vec[:, 300:428] = inp["gdn_norm_w"][l][None, :]
    m["vec"] = vec
    m["cstf"] = L.cstf_np; m["cstb"] = L.cstb_np
    return m

def _l2_weights(inp, l):
    w_in = inp["w_in"][l]
    m = {}
    m["wgate"] = np.stack([_wt(w_in[:, 20512 + i * 4096: 20512 + (i + 1) * 4096], 32, 32) for i in range(3)])
    m["wbr"] = np.stack([_wt(inp[k][l], 16, 32) for k in ("w_branch_sba", "w_branch_gdn", "w_branch_diff")])
    m["wout"] = _wt(inp["w_out"][l], 32, 32)
    lnv = np.zeros((128, 64), np.float32)
    lnv[:, 0:32] = np.asarray(inp["ln1_g"][l]).reshape(32, 128).T; lnv[:, 32:64] = np.asarray(inp["ln1_b"][l]).reshape(32, 128).T
    m["lnv"] = lnv; m["ones"] = np.ones((128, 128), np.float32)
    return m

def _l3_weights(inp, l):
    m = {}
    wr = np.concatenate([np.asarray(inp["w_router_group"][l]), np.asarray(inp["w_router_expert"][l])], axis=1)
    m["wr"] = np.ascontiguousarray(wr.reshape(32, 128, 36).transpose(1, 0, 2))
    rb = np.concatenate([np.asarray(inp["b_router_group"][l]), np.asarray(inp["b_router_expert"][l])])
    m["rb"] = np.ascontiguousarray(np.broadcast_to(rb[None, :], (128, 36))).astype(np.float32)
    m["sel"] = make_sel(); m["ident"] = np.eye(128, dtype=np.float32); m["ones"] = np.ones((128, 128), np.float32)
    def gu(W):
        return np.ascontiguousarray(np.asarray(W).reshape(32, 32, 128, 4, 128).transpose(0, 3, 2, 1, 4))
    m["wg"] = gu(inp["w_expert_gate"][l]); m["wu"] = gu(inp["w_expert_up"][l])
    m["wd"] = np.ascontiguousarray(np.asarray(inp["w_expert_down"][l]).reshape(32, 4, 128, 4096).transpose(0, 2, 1, 3))
    lnv = np.zeros((128, 64), np.float32)
    lnv[:, 0:32] = np.asarray(inp["ln2_g"][l]).reshape(32, 128).T; lnv[:, 32:64] = np.asarray(inp["ln2_b"][l]).reshape(32, 128).T
    m["lnv"] = lnv
    return m

def kernel(**inputs):
    inp = {k: np.asarray(v) for k, v in inputs.items()}
    x = inp["x"]
    B, S, Dm = x.shape
    NCORES = 8
    TC = B * S // NCORES
    per_b = S // TC
    TT2 = 256
    cores = list(range(NCORES))
    xT_all = np.ascontiguousarray(x.transpose(0, 2, 1).reshape(B, 32, 128, S).transpose(0, 2, 1, 3))
    for l in range(2):
        ua = [(k, hs, b) for k in ("sba", "diff") for hs in range(2) for b in range(B)]
        ub = [("gdn", hs, b) for hs in range(2) for b in range(B)]
        La = _prog(("l1a", S, B), lambda: (lambda L: (L, L.build()))(L1(S, B, ua)))
        Lb = _prog(("l1b", S, B), lambda: (lambda L: (L, L.build()))(L1(S, B, ub)))
        ra = run_bass_kernel_spmd(La[1], [_l1_inputs(inp, l, c, La[0], xT_all, S) for c in cores], core_ids=cores).results
        rb = run_bass_kernel_spmd(Lb[1], [_l1_inputs(inp, l, c, Lb[0], xT_all, S) for c in cores], core_ids=cores).results
        P2 = _prog(("l2", TC, TT2), lambda: L2(TC, TT2).build())
        w2 = _l2_weights(inp, l)
        ims = []
        for c in cores:
            b = c // per_b; s0 = (c % per_b) * TC
            yT = np.empty((3, 128, 16, TC), np.float32)
            for h in range(16):
                yT[0][:, h, :] = ra[h // 2]["ysba"][h % 2, b, :, s0:s0 + TC]
                yT[1][:, h, :] = rb[h // 2]["ygdn"][h % 2, b, s0:s0 + TC, :].T
                yT[2][:, h, :] = ra[h // 2]["ydiff"][h % 2, b, :, s0:s0 + TC]
            m = dict(w2); m["xT"] = np.ascontiguousarray(xT_all[b, :, :, s0:s0 + TC]); m["yT"] = yT
            ims.append(m)
        r2 = run_bass_kernel_spmd(P2, ims, core_ids=cores).results
        del ims, ra, rb, w2
        P3 = _prog(("l3", TC, TT2), lambda: L3(TC, TT2).build())
        w3 = _l3_weights(inp, l)
        ims = []
        for c in cores:
            m = dict(w3); m["x1T"] = r2[c]["x1T"]; ims.append(m)
        r3 = run_bass_kernel_spmd(P3, ims, core_ids=cores).results
        del ims, w3, r2
        xT_all = np.empty((B, 128, 32, S), np.float32)
        for c in cores:
            b = c // per_b; s0 = (c % per_b) * TC
            xT_all[b, :, :, s0:s0 + TC] = r3[c]["x2T"]
        del r3
    out = np.ascontiguousarray(xT_all.transpose(0, 3, 2, 1).reshape(B, S, Dm))
    return out.astype(np.float32)
```

```python
import math
import numpy as np
from contextlib import ExitStack
from concourse.bass_utils import run_bass_kernel_spmd
import concourse.bass as bass
import concourse.mybir as mybir
F32 = mybir.dt.float32; BF16 = mybir.dt.bfloat16; I32 = mybir.dt.int32
AF = mybir.ActivationFunctionType
ALU = mybir.AluOpType
AX = mybir.AxisListType

class Buf:
    __slots__ = ("name", "w", "r", "dsem", "dcnt")
    def __init__(self, name):
        self.name = name; self.w = None; self.r = {}; self.dsem = None; self.dcnt = 0

class MK:
    ENGS = ("pe", "act", "dve", "pool", "sp")
    def __init__(self, nc, stack):
        self.nc = nc; self.stack = stack
        self.ops = {e: [] for e in self.ENGS}
        self.sem = {}
        for e in ("pe", "act", "dve", "pool"):
            self.sem[e] = stack.enter_context(nc.semaphore("sem_" + e))
        self.cnt = {e: 0 for e in self.ENGS}
        self.seen = {e: {} for e in self.ENGS}
        self.semobj = dict(self.sem)
        self.nd = 0
        self.all_tokens = []
    def buf(self, name="b"):
        return Buf(name)
    def _need(self, eng, reads, writes, skip_self=False):
        need = {}
        def add(tok):
            if tok is None: return
            k, v = tok
            if skip_self and k == eng: return
            if need.get(k, 0) < v: need[k] = v
        if getattr(self, "serial", False):
            for k in ("pe", "act", "dve", "pool"):
                if self.cnt[k] > 0: add((k, self.cnt[k]))
            for k, v in getattr(self, "dlast", {}).items(): add((k, v))
        for b in reads: add(b.w)
        for b in writes:
            add(b.w)
            for k, v in b.r.items(): add((k, v))
        seen = self.seen[eng]
        for k, v in need.items():
            if seen.get(k, 0) >= v: continue
            seen[k] = v
            so = self.semobj[k]
            self.ops[eng].append(lambda e, so=so, v=v: e.wait_ge(so, v))
    def _done(self, tok, reads, writes):
        k, v = tok
        for b in reads:
            if b.r.get(k, 0) < v: b.r[k] = v
        for b in writes:
            b.w = tok; b.r = {}
    def op(self, eng, fn, reads=(), writes=(), skip_self=False, fence=False):
        self._need(eng, reads, writes, skip_self)
        self.cnt[eng] += 1
        idx = self.cnt[eng]
        so = self.sem[eng]
        self.ops[eng].append(lambda e, fn=fn, so=so: fn(e).then_inc(so, 1))
        if (fence or eng in getattr(self, "fence_engs", ())) and getattr(self, "fence_ap", None) is not None:
            self.cnt[eng] += 1
            idx = self.cnt[eng]
            fa = self.fence_ap
            if eng == "act":
                self.ops[eng].append(lambda e, fa=fa, so=so: e.memzero(fa).then_inc(so, 1))
            else:
                self.ops[eng].append(lambda e, fa=fa, so=so: e.memset(fa, 0.0).then_inc(so, 1))
        self.seen[eng][eng] = max(self.seen[eng].get(eng, 0), 0)
        self._done((eng, idx), reads, writes)
    def do(self, eng, method, *args, reads=(), writes=(), skip_self=False, **kw):
        self.op(eng, lambda e, method=method, args=args, kw=kw: getattr(e, method)(*args, **kw), reads=reads, writes=writes, skip_self=skip_self,
                fence=(eng == "dve" and method == "scalar_tensor_tensor"))
    def dma(self, q, out, in_, reads=(), writes=(), sembuf=None, **kw):
        if sembuf is None:
            sembuf = (list(writes) + list(reads))[0]
        if sembuf.dsem is None:
            self.nd += 1
            sembuf.dsem = "d%d_%s" % (self.nd, sembuf.name)
            self.semobj[sembuf.dsem] = self.stack.enter_context(self.nc.semaphore(sembuf.dsem))
        self._need(q, reads, writes)
        sembuf.dcnt += 16
        so = self.semobj[sembuf.dsem]
        self.ops[q].append(lambda e, out=out, in_=in_, so=so, kw=kw: e.dma_start(out=out, in_=in_, **kw).then_inc(so, 16))
        if not hasattr(self, "dlast"): self.dlast = {}
        self.dlast[sembuf.dsem] = sembuf.dcnt
        self._done((sembuf.dsem, sembuf.dcnt), reads, writes)
        return (sembuf.dsem, sembuf.dcnt)
    def barrier(self):
        toks = [(k, self.cnt[k]) for k in ("pe", "act", "dve", "pool") if self.cnt[k] > 0]
        toks += list(getattr(self, "dlast", {}).items())
        for eng in self.ENGS:
            seen = self.seen[eng]
            for k, v in toks:
                if seen.get(k, 0) >= v: continue
                seen[k] = v
                so = self.semobj[k]
                self.ops[eng].append(lambda e, so=so, v=v: e.wait_ge(so, v))
    def finish(self, final_bufs):
        need = {}
        for b in final_bufs:
            for tok in [b.w] + list(b.r.items()):
                if tok is None: continue
                k, v = tok
                need[k] = max(need.get(k, 0), v)
        for k, v in need.items():
            so = self.semobj[k]
            self.ops["sp"].append(lambda e, so=so, v=v: e.wait_ge(so, v))
        nc = self.nc
        ops = self.ops
        with nc.Block() as block:
            @block.sync
            def _(e):
                for f in ops["sp"]: f(e)
            @block.tensor
            def _(e):
                for f in ops["pe"]: f(e)
            @block.scalar
            def _(e):
                for f in ops["act"]: f(e)
            @block.vector
            def _(e):
                for f in ops["dve"]: f(e)
            @block.gpsimd
            def _(e):
                for f in ops["pool"]: f(e)


TT = 512
LN_EPS = 1e-5
NORM_EPS = 1e-6

def make_cst():
    c = {}
    p = np.arange(128)[:, None]; j = np.arange(128)[None, :]
    c["ident"] = (p == j).astype(np.float32)
    c["ones"] = np.ones((128, 128), np.float32)
    c["negtri"] = -(p >= j).astype(np.float32)
    c["negones"] = -np.ones((128, 128), np.float32)
    jj = np.arange(512)[None, :]
    for i in range(4):
        c["mstrict%d" % i] = ((i * 128 + p) < jj).astype(np.float32)
        c["mincl%d" % i] = ((i * 128 + p) <= jj).astype(np.float32)
    same = (p // 64) == (j // 64)
    NEG = -30000.0
    c["bt"] = (same & (p <= j)).astype(np.float32)
    c["negm_strict"] = np.where(same & (p > j), 0.0, NEG).astype(np.float32)
    c["negm_inclT"] = np.where(same & (j >= p), 0.0, NEG).astype(np.float32)
    fn = ["ident", "ones", "bt", "negm_strict", "negm_inclT"]
    bn = ["ones", "negtri", "negones"] + ["mstrict%d" % i for i in range(4)] + ["mincl%d" % i for i in range(4)]
    def pack(names):
        offs = {}; o = 0
        for n in names:
            offs[n] = (o, c[n].shape[1]); o += c[n].shape[1]
        return np.ascontiguousarray(np.concatenate([c[n] for n in names], axis=1)), offs
    af, of = pack(fn); ab, ob = pack(bn)
    return af, of, ab, ob

def make_rope(S):
    pos = np.arange(S, dtype=np.float32)
    inv_freq = (10000.0 ** (-np.arange(0, 64, 2, dtype=np.float32) / 64)).astype(np.float32)
    ang = pos[None, :] * inv_freq[:, None]
    cos = np.cos(ang).astype(np.float32); sin = np.sin(ang).astype(np.float32)
    cosT = np.concatenate([cos, cos, cos, cos], axis=0)
    sinS = np.concatenate([sin, -sin, sin, -sin], axis=0)
    return cosT, sinS

class L1:
    def __init__(self, S, B, units):
        self.S, self.B, self.units = S, B, units
        self.NT = S // TT; self.NB = S // 128
        self.cstf_np, self.cofff, self.cstb_np, self.coffb = make_cst()
        self.NCF = self.cstf_np.shape[1]; self.NCB = self.cstb_np.shape[1]
        self.kinds = set(k for (k, _, _) in units)

    def build(self):
        S, B = self.S, self.B
        nc = bass.Bass("TRN2", target_bir_lowering=False)
        self.nc = nc
        D = lambda n, s, kind: nc.dram_tensor(n, s, F32, kind=kind).ap()
        self.xT = D("xT", [B, 128, 32, S], "ExternalInput")
        kinds = self.kinds
        if "sba" in kinds:
            self.wsba = D("wsba", [2, 3, 128, 32, 128], "ExternalInput")
            self.ysba = D("ysba", [2, B, 128, S], "ExternalOutput")
        if "diff" in kinds:
            self.wdiff = D("wdiff", [2, 3, 128, 32, 128], "ExternalInput")
            self.ropec = D("ropec", [128, S], "ExternalInput")
            self.ropes = D("ropes", [128, S], "ExternalInput")
            self.ydiff = D("ydiff", [2, B, 128, S], "ExternalOutput")
        if "gdn" in kinds:
            self.wgdn = D("wgdn", [2, 4, 128, 32, 128], "ExternalInput")
            self.wgab = D("wgab", [128, 32, 4], "ExternalInput")
            self.ygdn = D("ygdn", [2, B, S, 128], "ExternalOutput")
        self.vec = D("vec", [128, 1024], "ExternalInput")
        self.cstf = D("cstf", [128, self.NCF], "ExternalInput")
        self.cstb = D("cstb", [128, self.NCB], "ExternalInput")
        with ExitStack() as st:
            self.st = st
            mk = MK(nc, st); self.mk = mk
            self.alloc()
            self.load_consts()
            for (kind, hs, b) in self.units:
                getattr(self, "unit_" + kind)(hs, b)
            for (name, t, tb) in getattr(self, "dbg", []):
                shp = list(t.shape)
                d = nc.dram_tensor("dbg_" + name, shp, F32, kind="ExternalOutput").ap()
                db = mk.buf("dbg_" + name)
                mk.dma("pool", d, t[:], reads=[tb], writes=[db])
                self.outbufs.append(db)
            mk.finish(self.outbufs)
        return nc

    def probe(self, name, t, tb):
        if not getattr(self, "probing", False): return
        shp = list(t.shape)
        d = self.nc.dram_tensor("dbg_" + name, shp, F32, kind="ExternalOutput").ap()
        db = self.mk.buf("dbg_" + name)
        self.mk.dma("pool", d, t[:], reads=[tb], writes=[db])
        self.outbufs.append(db)

    def T(self, name, shape, dt=F32):
        t = self.st.enter_context(self.nc.sbuf_tensor(name, shape, dt))
        return t, self.mk.buf(name)

    def alloc(self):
        nc, mk, st = self.nc, self.mk, self.st
        S, NB = self.S, self.NB
        self.ps = []; self.pb = []
        for i in range(8):
            self.ps.append(st.enter_context(nc.psum_tensor("ps%d" % i, [128, 512], F32)))
            self.pb.append(mk.buf("ps%d" % i))
        self.xt = [self.T("xt%d" % i, [128, 32, TT], BF16) for i in range(1 if self.kinds == {"gdn"} else 2)]
        self.w = [self.T("w%d" % i, [128, 32, 128], BF16) for i in range(4)]
        self.wab = self.T("wab", [128, 32, 4], BF16)
        self.cf = self.T("cf", [128, self.NCF], F32)
        self.cb = self.T("cb", [128, self.NCB], BF16)
        self.vc = self.T("vc", [128, 1024], F32)
        self.sm = self.T("sm", [128, 64], F32)
        self.fence_t = self.T("fence_t", [128, 4], F32)
        mk.fence_ap = self.fence_t[0][:, 0:1]
        import os
        mk.fence_engs = tuple(x for x in os.environ.get("FENCE", "").split(",") if x)
        self.epst = self.T("epst", [128, 4], F32)
        mk.op("dve", lambda e: e.memset(self.epst[0][:, 0:1], LN_EPS), writes=[self.epst[1]])
        mk.op("dve", lambda e: e.memset(self.epst[0][:, 1:2], NORM_EPS), writes=[self.epst[1]])
        self.Ysba = mk.buf("Ysba"); self.Ydiff = mk.buf("Ydiff"); self.Ygdn = mk.buf("Ygdn")
        self.outbufs = [self.Ysba, self.Ydiff, self.Ygdn]
        self.xt_cnt = 0
        if not (self.kinds & {"sba", "diff"}):
            return
        self.QT = self.T("QT", [128, S], BF16)
        self.KT = self.T("KT", [128, S], BF16)
        self.V = self.T("V", [128, NB, 128], BF16)
        self.qtb = [mk.buf("qt%d" % i) for i in range(self.NT)]
        self.ktb = [mk.buf("kt%d" % i) for i in range(self.NT)]
        self.vtb = [mk.buf("vt%d" % i) for i in range(self.NT)]
        self.Et = [self.T("Et%d" % i, [128, TT], F32) for i in range(2)]
        self.SPb = [self.T("SPb%d" % i, [128, TT], BF16) for i in range(2)]
        self.att = [self.T("att%d" % i, [128, TT], BF16) for i in range(3)]
        self.SPacc = self.T("SPacc", [128, TT], F32)
        self.SPaccb = [self.T("SPaccb%d" % i, [128, TT], BF16) for i in range(2)]
        self.ost = [self.T("ost%d" % i, [128, TT], F32) for i in range(2)]
        self.f32t = [self.T("f32t%d" % i, [128, TT], F32) for i in range(6)]
        self.rc = [self.T("rc%d" % i, [128, TT], F32) for i in range(2)]
        self.rs = [self.T("rs%d" % i, [128, TT], F32) for i in range(2)]

    def c_f(self, name, cols=None):
        o, w = self.cofff[name]
        return self.cf[0][:, o:o + (cols or w)]

    def c_b(self, name, cols=None):
        o, w = self.coffb[name]
        return self.cb[0][:, o:o + (cols or w)]

    def load_consts(self):
        mk = self.mk
        mk.dma("sp", self.cf[0][:], self.cstf, writes=[self.cf[1]])
        mk.dma("pool", self.cb[0][:], self.cstb, writes=[self.cb[1]])
        mk.dma("sp", self.vc[0][:], self.vec, writes=[self.vc[1]])

    def load_w(self, src, n):
        for i in range(n):
            self.mk.dma("pool", self.w[i][0][:], src[i], writes=[self.w[i][1]])

    def load_xt(self, b, t):
        s = self.xt_cnt % len(self.xt); self.xt_cnt += 1
        xt, xb = self.xt[s]
        self.mk.dma("pool", xt[:], self.xT[b, :, :, t * TT:(t + 1) * TT], writes=[xb])
        return xt, xb

    def proj_fm(self, xt, xb, wi, bank):
        mk = self.mk; w, wb = self.w[wi]; ps, pb = self.ps[bank], self.pb[bank]
        for k in range(32):
            mk.op("pe", lambda e, k=k: e.matmul(ps[:], w[:, k, :], xt[:, k, :], start=(k == 0), stop=(k == 31)),
                  reads=[wb, xb], writes=[pb], skip_self=True)

    def proj_tm(self, xt, xb, wi, bank):
        mk = self.mk; w, wb = self.w[wi]; ps, pb = self.ps[bank], self.pb[bank]
        for blk in range(4):
            for k in range(32):
                mk.op("pe", lambda e, k=k, blk=blk: e.matmul(ps[:, blk * 128:(blk + 1) * 128], xt[:, k, blk * 128:(blk + 1) * 128], w[:, k, :],
                                                             start=(k == 0), stop=(k == 31)),
                      reads=[wb, xb], writes=[pb], skip_self=True)

    def unit_sba(self, hs, b):
        mk = self.mk; NT = self.NT
        self.load_w(self.wsba[hs], 3)
        scale = 128 ** -0.5
        QT, _ = self.QT; KT, _ = self.KT; V, _ = self.V
        def proj(t):
            xt, xb = self.load_xt(b, t)
            self.proj_fm(xt, xb, 0, 5)
            mk.op("act", lambda e: e.activation(QT[:, t * TT:(t + 1) * TT], self.ps[5][:], AF.Copy, scale=scale), reads=[self.pb[5]], writes=[self.qtb[t]])
            self.proj_fm(xt, xb, 1, 6)
            mk.op("dve", lambda e: e.tensor_copy(KT[:, t * TT:(t + 1) * TT], self.ps[6][:]), reads=[self.pb[6]], writes=[self.ktb[t]])
            self.proj_tm(xt, xb, 2, 7)
            mk.op("dve", lambda e: e.tensor_copy(V[:, 4 * t:4 * t + 4, :], self.ps[7][:].rearrange("p (a d) -> p a d", a=4)), reads=[self.pb[7]], writes=[self.vtb[t]])
        proj(0)
        for qt in range(NT):
            if qt + 1 < NT:
                proj(qt + 1)
            self.sba_attn(hs, b, qt)

    def sba_attn(self, hs, b, qt):
        mk = self.mk
        QT, _ = self.QT; KT, _ = self.KT; V, _ = self.V
        nkb = 4 * qt + 4
        kbs = list(range(nkb - 1, -1, -1))
        qs = slice(qt * TT, (qt + 1) * TT)
        psC, pbC = self.ps[4], self.pb[4]
        SPacc, SPaccB = self.SPacc
        def s1(idx):
            kb = kbs[idx]; a = idx % 2; i = kb - 4 * qt
            psA, pbA = self.ps[a], self.pb[a]
            Et, Etb = self.Et[a]; SPb, SPbb = self.SPb[a]
            ks = slice(kb * 128, (kb + 1) * 128)
            mk.op("pe", lambda e: e.matmul(psA[:], KT[:, ks], QT[:, qs], start=True, stop=True),
                  reads=[self.ktb[kb // 4], self.qtb[qt]], writes=[pbA], skip_self=True)
            mk.op("act", lambda e: e.activation(Et[:], psA[:], AF.Exp), reads=[pbA], writes=[Etb])
            mk.op("act", lambda e: e.activation(SPb[:], Et[:], AF.Ln, bias=1.0), reads=[Etb], writes=[SPbb])
            if i >= 0:
                m = self.c_b("mstrict%d" % i)
                mk.op("dve", lambda e: e.tensor_tensor(SPb[:], SPb[:], m, ALU.mult), reads=[SPbb, self.cb[1]], writes=[SPbb])
        def s2(idx):
            kb = kbs[idx]; a = idx % 2; i = kb - 4 * qt
            psB, pbB = self.ps[2 + a], self.pb[2 + a]
            SPb, SPbb = self.SPb[a]
            att, attb = self.att[idx % 3]
            ks = slice(kb * 128, (kb + 1) * 128)
            last = (idx == 0)
            mk.op("pe", lambda e: e.matmul(psB[:], KT[:, ks], QT[:, qs], start=True, stop=False),
                  reads=[self.ktb[kb // 4], self.qtb[qt]], writes=[pbB], skip_self=True)
            mk.op("pe", lambda e: e.matmul(psB[:], self.c_b("negtri"), SPb[:], start=False, stop=last),
                  reads=[SPbb, self.cb[1]], writes=[pbB], skip_self=True)
            if idx > 0:
                sab, sabb = self.SPaccb[(idx - 1) % 2]
                mk.op("pe", lambda e: e.matmul(psB[:], self.c_b("negones"), sab[:], start=False, stop=True),
                      reads=[sabb, self.cb[1]], writes=[pbB], skip_self=True)
            mk.op("act", lambda e: e.activation(att[:], psB[:], AF.Exp), reads=[pbB], writes=[attb])
            if i >= 0:
                m = self.c_b("mstrict%d" % i)
                mk.op("dve", lambda e: e.tensor_tensor(att[:], att[:], m, ALU.mult), reads=[attb, self.cb[1]], writes=[attb])
            if qt == 0:
                self.probe("SP%d" % idx, SPb, SPbb); self.probe("att%d" % idx, att, attb)
                if idx > 0: self.probe("sab%d" % idx, self.SPaccb[(idx - 1) % 2][0], self.SPaccb[(idx - 1) % 2][1])
            mk.op("pe", lambda e: e.matmul(psC[:], V[:, kb, :], att[:], start=(idx == 0), stop=(idx == nkb - 1)),
                  reads=[self.vtb[kb // 4], attb], writes=[pbC], skip_self=True)
            if idx < nkb - 1:
                if idx == 0:
                    mk.op("pool", lambda e: e.tensor_copy(SPacc[:], SPb[:]), reads=[SPbb], writes=[SPaccB])
                else:
                    mk.op("pool", lambda e: e.tensor_tensor(SPacc[:], SPacc[:], SPb[:], ALU.add), reads=[SPbb, SPaccB], writes=[SPaccB])
                sab2, sabb2 = self.SPaccb[idx % 2]
                mk.op("dve", lambda e: e.tensor_copy(sab2[:], SPacc[:]), reads=[SPaccB], writes=[sabb2])
        s1(0)
        for idx in range(nkb):
            if idx + 1 < nkb:
                s1(idx + 1)
            s2(idx)
        ost, ostb = self.ost[qt % 2]
        mk.op("dve", lambda e: e.tensor_copy(ost[:], psC[:]), reads=[pbC], writes=[ostb])
        mk.dma("sp", self.ysba[hs, b, :, qs], ost[:], reads=[ostb], writes=[self.Ysba], sembuf=ostb)

    def unit_diff(self, hs, b):
        mk = self.mk; NT = self.NT
        self.load_w(self.wdiff[hs], 3)
        QT, _ = self.QT; KT, _ = self.KT; V, _ = self.V
        vc, vcb = self.vc
        sm, smb = self.sm
        t0, t0b = self.f32t[0]
        mk.op("dve", lambda e: e.tensor_tensor(t0[:, 0:64], vc[:, 0:64], vc[:, 64:128], ALU.mult), reads=[vcb], writes=[t0b])
        mk.op("dve", lambda e: e.tensor_reduce(sm[:, 0:1], t0[:, 0:64], AX.X, ALU.add), reads=[t0b], writes=[smb])
        mk.op("dve", lambda e: e.tensor_tensor(t0[:, 64:128], vc[:, 128:192], vc[:, 192:256], ALU.mult), reads=[vcb], writes=[t0b])
        mk.op("dve", lambda e: e.tensor_reduce(sm[:, 1:2], t0[:, 64:128], AX.X, ALU.add), reads=[t0b], writes=[smb])
        mk.op("act", lambda e: e.activation(sm[:, 2:4], sm[:, 0:2], AF.Exp), reads=[smb], writes=[smb])
        mk.op("dve", lambda e: e.tensor_tensor(sm[:, 4:5], sm[:, 3:4], sm[:, 2:3], ALU.subtract), reads=[smb], writes=[smb])
        mk.op("dve", lambda e: e.tensor_tensor(sm[:, 5:6], sm[:, 4:5], vc[:, 256:257], ALU.subtract), reads=[smb, vcb], writes=[smb])
        mk.op("dve", lambda e: e.tensor_tensor(sm[:, 6:7], vc[:, 258:259], vc[:, 257:258], ALU.mult), reads=[smb, vcb], writes=[smb])
        def rope(src_ps, src_pb, dst, dstb, t, scl):
            qf, qfb = self.f32t[1]; A, Ab = self.f32t[2]; Bm, Bb = self.f32t[3]
            rcs, rcb = self.rc[t % 2]; rss, rsb = self.rs[t % 2]
            mk.op("act", lambda e: e.activation(qf[:], src_ps[:], AF.Copy, scale=scl), reads=[src_pb], writes=[qfb])
            mk.op("dve", lambda e: e.tensor_tensor(A[:], qf[:], rcs[:], ALU.mult), reads=[qfb, rcb], writes=[Ab])
            for (o, s_) in ((0, 32), (32, 0), (64, 96), (96, 64)):
                mk.op("pool", lambda e, o=o, s_=s_: e.tensor_tensor(Bm[o:o + 32, :], qf[s_:s_ + 32, :], rss[s_:s_ + 32, :], ALU.mult),
                      reads=[qfb, rsb], writes=[Bb])
            mk.op("dve", lambda e: e.tensor_tensor(dst[:, t * TT:(t + 1) * TT], A[:], Bm[:], ALU.add), reads=[Ab, Bb], writes=[dstb])
        def proj(t):
            xt, xb = self.load_xt(b, t)
            rcs, rcb = self.rc[t % 2]; rss, rsb = self.rs[t % 2]
            mk.dma("sp", rcs[:], self.ropec[:, t * TT:(t + 1) * TT], writes=[rcb])
            mk.dma("sp", rss[:], self.ropes[:, t * TT:(t + 1) * TT], writes=[rsb])
            self.proj_fm(xt, xb, 0, 6)
            rope(self.ps[6], self.pb[6], QT, self.qtb[t], t, 0.125)
            self.proj_fm(xt, xb, 1, 7)
            rope(self.ps[7], self.pb[7], KT, self.ktb[t], t, 1.0)
            self.proj_tm(xt, xb, 2, 6)
            mk.op("dve", lambda e: e.tensor_copy(V[:, 4 * t:4 * t + 4, :], self.ps[6][:].rearrange("p (a d) -> p a d", a=4)), reads=[self.pb[6]], writes=[self.vtb[t]])
        proj(0)
        for qt in range(NT):
            if qt + 1 < NT:
                proj(qt + 1)
            self.diff_attn(hs, b, qt)

    def diff_attn(self, hs, b, qt):
        mk = self.mk
        QT, _ = self.QT; KT, _ = self.KT; V, _ = self.V
        sm, smb = self.sm
        nkb = 4 * qt + 4
        qs = slice(qt * TT, (qt + 1) * TT)
        seq = [(kb, c) for kb in range(nkb) for c in range(2)]
        n = len(seq)
        def s1(idx):
            kb, c = seq[idx]; a = idx % 2; i = kb - 4 * qt
            psA, pbA = self.ps[a], self.pb[a]
            P, Pb = self.att[idx % 3]
            ks = slice(kb * 128, (kb + 1) * 128); cs = slice(64 * c, 64 * c + 64)
            mk.op("pe", lambda e: e.matmul(psA[:], KT[cs, ks], QT[cs, qs], start=True, stop=True),
                  reads=[self.ktb[kb // 4], self.qtb[qt]], writes=[pbA], skip_self=True)
            mk.op("act", lambda e: e.activation(P[:], psA[:], AF.Exp), reads=[pbA], writes=[Pb])
            if i >= 0:
                m = self.c_b("mincl%d" % i)
                mk.op("dve", lambda e: e.tensor_tensor(P[:], P[:], m, ALU.mult), reads=[Pb, self.cb[1]], writes=[Pb])
        def s2(idx):
            kb, c = seq[idx]
            P, Pb = self.att[idx % 3]
            psO, pbO = self.ps[2 + c], self.pb[2 + c]
            psL, pbL = self.ps[4 + c], self.pb[4 + c]
            first = (kb == 0); last = (kb == nkb - 1)
            mk.op("pe", lambda e: e.matmul(psO[:], V[:, kb, :], P[:], start=first, stop=last),
                  reads=[self.vtb[kb // 4], Pb], writes=[pbO], skip_self=True)
            mk.op("pe", lambda e: e.matmul(psL[:], self.c_b("ones"), P[:], start=first, stop=last),
                  reads=[self.cb[1], Pb], writes=[pbL], skip_self=True)
        s1(0)
        for idx in range(n):
            if idx + 1 < n:
                s1(idx + 1)
            s2(idx)
        r, rb = self.f32t[4]; o0, o0b = self.f32t[5]; o1, o1b = self.f32t[0]
        mk.op("dve", lambda e: e.reciprocal(r[:], self.ps[4][:]), reads=[self.pb[4]], writes=[rb])
        mk.op("dve", lambda e: e.tensor_tensor(o0[:], self.ps[2][:], r[:], ALU.mult), reads=[self.pb[2], rb], writes=[o0b])
        mk.op("dve", lambda e: e.reciprocal(r[:], self.ps[5][:]), reads=[self.pb[5]], writes=[rb])
        mk.op("dve", lambda e: e.tensor_tensor(o1[:], self.ps[3][:], r[:], ALU.mult), reads=[self.pb[3], rb], writes=[o1b])
        mk.op("dve", lambda e: e.scalar_tensor_tensor(o0[:], o1[:], sm[:, 5:6], o0[:], ALU.mult, ALU.add), reads=[o1b, o0b, smb], writes=[o0b])
        sq, sqb = self.Et[0]
        mk.op("act", lambda e: e.activation(sq[:], o0[:], AF.Square), reads=[o0b], writes=[sqb])
        psS, pbS = self.ps[0], self.pb[0]
        mk.op("pe", lambda e: e.matmul(psS[:], self.c_f("ones"), sq[:], start=True, stop=True), reads=[self.cf[1], sqb], writes=[pbS], skip_self=True)
        mk.op("act", lambda e: e.activation(r[:], psS[:], AF.Ln, bias=self.epst[0][:, 0:1], scale=1.0 / 128), reads=[pbS, self.epst[1]], writes=[rb])
        mk.op("act", lambda e: e.activation(r[:], r[:], AF.Exp, scale=-0.5), reads=[rb], writes=[rb])
        ost, ostb = self.ost[qt % 2]
        mk.op("dve", lambda e: e.scalar_tensor_tensor(ost[:], o0[:], sm[:, 6:7], r[:], ALU.mult, ALU.mult), reads=[o0b, rb, smb], writes=[ostb])
        mk.dma("sp", self.ydiff[hs, b, :, qs], ost[:], reads=[ostb], writes=[self.Ydiff], sembuf=ostb)

    def gdn_alloc(self):
        if hasattr(self, "g_done"): return
        self.g_done = True
        mk = self.mk
        T = self.T
        self.g_raw = [[T("graw%d_%d" % (m, p), [128, 3 + TT], F32) for p in range(2)] for m in range(3)]
        self.g_cv = [T("gcv%d" % m, [128, TT], F32) for m in range(3)]
        self.g_tmp = [T("gtmp%d" % m, [128, TT], F32) for m in range(4)]
        self.g_zs = T("gzs", [128, 4, 128], F32)
        self.g_nwz = T("gnwz", [128, 4, 128], F32)
        self.g_ktm = T("gktm", [128, 4, 128], F32)
        self.g_vtm = T("gvtm", [128, 4, 128], F32)
        self.g_ab = T("gab", [128, 8], F32)
        self.g_s = T("gs", [128, 64], F32)
        self.g_sm = T("gsm", [128, 8], F32)
        self.g_D4 = T("gD4", [128, 4, 128], F32)
        self.g_gcB = T("ggcB", [128, 4, 128], F32)
        self.Sst = T("Sst", [128, 128], F32)
        names = ["t1", "dec", "t2", "decT", "M", "N", "X0", "X1", "P0", "P1", "PT0", "PT1", "intraT", "vb", "kbg", "u", "wT", "egcB", "qdT", "kdec", "vnew", "ost", "o", "sq", "y"]
        self.g_blk = [{n: T("g_%s_%d" % (n, s), [128, 128], F32) for n in names} for s in range(4)]
        self.g_ed = T("ged", [128, 4], F32)
        self.g_cd = T("gcd", [128, 8], F32)
        self.g_r = T("gr", [128, 8], F32)
        self.qi = 0; self.wi_ = 0

    def qg(self):
        i = self.qi % 4; self.qi += 1
        bank = self.ps[4 + i]; tok = self.pb[4 + i]
        return [(bank[:, q * 128:(q + 1) * 128], tok) for q in range(4)]

    def qb(self):
        return self.qg()[0]

    def wbk(self):
        i = self.wi_ % 4; self.wi_ += 1
        return self.ps[i], self.pb[i]

    def unit_gdn(self, hs, b):
        mk = self.mk; do = mk.do
        self.gdn_alloc()
        mk.barrier()
        self.load_w(self.wgdn[hs], 4)
        wab, wabb = self.wab
        mk.dma("pool", wab[:], self.wgab, writes=[wabb])
        vc, vcb = self.vc
        gsm, gsmb = self.g_sm
        do("act", "activation", gsm[:, 0:1], vc[:, 290 + hs:291 + hs], AF.Exp, reads=[vcb], writes=[gsmb])
        do("dve", "tensor_scalar", gsm[:, 0:1], gsm[:, 0:1], -1.0, None, ALU.mult, reads=[gsmb], writes=[gsmb])
        Sst, Sstb = self.Sst
        do("dve", "memset", Sst[:], 0.0, writes=[Sstb])
        for m in range(3):
            raw, rawb = self.g_raw[m][1]
            do("pool", "memset", raw[:, TT:TT + 3], 0.0, writes=[rawb])
        for t in range(self.NT):
            self.gdn_tile(hs, b, t)

    def silu_inplace(self, x, xb, tmp, tmpb, eng2="pool"):
        do = self.mk.do
        do("act", "activation", tmp, x, AF.Exp, scale=-1.0, reads=[xb], writes=[tmpb])
        do(eng2, "tensor_scalar", tmp, tmp, 1.0, None, ALU.add, reads=[tmpb], writes=[tmpb])
        do("dve", "reciprocal", tmp, tmp, reads=[tmpb], writes=[tmpb])
        do(eng2, "tensor_tensor", x, x, tmp, ALU.mult, reads=[xb, tmpb], writes=[xb])

    def gdn_tile(self, hs, b, t):
        mk = self.mk; do = mk.do
        par = t % 2
        vc, vcb = self.vc
        cf, cfb = self.cf
        ident = self.c_f("ident"); ones = self.c_f("ones")
        xt, xb = self.load_xt(b, t)
        for m in range(3):
            ps, pb = self.wbk()
            w, wb = self.w[m]
            for k in range(32):
                do("pe", "matmul", ps[:], w[:, k, :], xt[:, k, :], start=(k == 0), stop=(k == 31), reads=[wb, xb], writes=[pb], skip_self=True)
            raw, rawb = self.g_raw[m][par]; praw, prawb = self.g_raw[m][1 - par]
            do("act" if m != 1 else "dve", "activation" if m != 1 else "tensor_copy", raw[:, 3:3 + TT], ps[:], *( [AF.Copy] if m != 1 else []), reads=[pb], writes=[rawb])
            do("pool", "tensor_copy", raw[:, 0:3], praw[:, TT:TT + 3], reads=[prawb], writes=[rawb])
        ps, pb = self.wbk()
        w, wb = self.w[3]
        for blk in range(4):
            for k in range(32):
                do("pe", "matmul", ps[:, blk * 128:(blk + 1) * 128], xt[:, k, blk * 128:(blk + 1) * 128], w[:, k, :], start=(k == 0), stop=(k == 31),
                   reads=[wb, xb], writes=[pb], skip_self=True)
        zs, zsb = self.g_zs
        do("act", "activation", zs[:].rearrange("p a d -> p (a d)"), ps[:], AF.Copy, reads=[pb], writes=[zsb])
        wab, wabb = self.wab
        pq, pqb = self.qb()
        for blk in range(4):
            for k in range(32):
                do("pe", "matmul", pq[:, blk * 2:blk * 2 + 2], xt[:, k, blk * 128:(blk + 1) * 128], wab[:, k, 2 * hs:2 * hs + 2], start=(k == 0), stop=(k == 31),
                   reads=[wabb, xb], writes=[pqb], skip_self=True)
        ab, abb = self.g_ab
        do("dve", "tensor_copy", ab[:], pq[:, 0:8], reads=[pqb], writes=[abb])
        if getattr(self, "stop_after", None) == "proj": return
        for m in range(3):
            raw, rawb = self.g_raw[m][par]
            cv, cvb = self.g_cv[m]
            eng = "dve"
            c0 = 260 + (hs * 3 + m) * 4
            do(eng, "tensor_scalar", cv[:], raw[:, 0:TT], vc[:, c0:c0 + 1], None, ALU.mult, reads=[rawb, vcb], writes=[cvb])
            for j in range(1, 4):
                do(eng, "scalar_tensor_tensor", cv[:], raw[:, j:j + TT], vc[:, c0 + j:c0 + j + 1], cv[:], ALU.mult, ALU.add, reads=[rawb, vcb, cvb], writes=[cvb])
            tmp, tmpb = self.g_tmp[m]
            self.silu_inplace(cv[:], cvb, tmp[:], tmpb)
        if getattr(self, "stop_after", None) == "conv": return
        for m in range(2):
            cv, cvb = self.g_cv[m]; tmp, tmpb = self.g_tmp[m]
            do("act", "activation", tmp[:], cv[:], AF.Square, reads=[cvb], writes=[tmpb])
            ps, pb = self.wbk()
            do("pe", "matmul", ps[:], ones, tmp[:], start=True, stop=True, reads=[cfb, tmpb], writes=[pb], skip_self=True)
            do("act", "activation", tmp[:], ps[:], AF.Ln, bias=self.epst[0][:, 1:2], reads=[pb, self.epst[1]], writes=[tmpb])
            do("act", "activation", tmp[:], tmp[:], AF.Exp, scale=-0.5, reads=[tmpb], writes=[tmpb])
            if m == 0:
                do("dve", "scalar_tensor_tensor", cv[:], cv[:], 128 ** -0.5, tmp[:], ALU.mult, ALU.mult, reads=[cvb, tmpb], writes=[cvb])
            else:
                do("dve", "tensor_tensor", cv[:], cv[:], tmp[:], ALU.mult, reads=[cvb, tmpb], writes=[cvb])
        qTn, qTnb = self.g_cv[0]; kTn, kTnb = self.g_cv[1]; vT, vTb = self.g_cv[2]
        if getattr(self, "stop_after", None) == "l2": return
        ktm, ktmb = self.g_ktm; vtm, vtmb = self.g_vtm
        for (src, srcb, dst, dstb, eng) in ((kTn, kTnb, ktm, ktmb, "act"), (vT, vTb, vtm, vtmb, "dve")):
            ps, pb = self.wbk()
            for blk in range(4):
                do("pe", "matmul", ps[:, blk * 128:(blk + 1) * 128], src[:, blk * 128:(blk + 1) * 128], ident, start=True, stop=True,
                   reads=[srcb, cfb], writes=[pb], skip_self=True)
            if eng == "act":
                do("act", "activation", dst[:].rearrange("p a d -> p (a d)"), ps[:], AF.Copy, reads=[pb], writes=[dstb])
            else:
                do("dve", "tensor_copy", dst[:].rearrange("p a d -> p (a d)"), ps[:], reads=[pb], writes=[dstb])
        if getattr(self, "stop_after", None) == "tr": return
        zs2 = zs[:].rearrange("p a d -> p (a d)")
        tmp, tmpb = self.g_tmp[3]
        self.silu_inplace(zs2, zsb, tmp[:], tmpb)
        nwz, nwzb = self.g_nwz
        for blk in range(4):
            do("pool", "tensor_tensor", nwz[:, blk, :], zs[:, blk, :], vc[:, 300:428], ALU.mult, reads=[zsb, vcb], writes=[nwzb])
        if getattr(self, "stop_after", None) == "zn": return
        gs, gsb = self.g_s
        gsm, gsmb = self.g_sm
        ab2 = ab[:].rearrange("p (k two) -> p two k", two=2)
        a4 = ab2[:, 0, :]; b4 = ab2[:, 1, :]
        do("act", "activation", gs[:, 28:32], a4, AF.Exp, bias=vc[:, 292 + hs:293 + hs], reads=[abb, vcb], writes=[gsb])
        do("act", "activation", gs[:, 28:32], gs[:, 28:32], AF.Ln, bias=1.0, reads=[gsb], writes=[gsb])
        do("dve", "tensor_scalar", gs[:, 0:4], gs[:, 28:32], gsm[:, 0:1], None, ALU.mult, reads=[gsb, gsmb], writes=[gsb])
        do("act", "activation", gs[:, 4:8], b4, AF.Exp, scale=-1.0, reads=[abb], writes=[gsb])
        do("dve", "tensor_scalar", gs[:, 4:8], gs[:, 4:8], 1.0, None, ALU.add, reads=[gsb], writes=[gsb])
        do("dve", "reciprocal", gs[:, 4:8], gs[:, 4:8], reads=[gsb], writes=[gsb])
        pq, pqb = self.qb()
        do("pe", "matmul", pq[:, 0:4], self.c_f("bt"), gs[:, 0:4], start=True, stop=True, reads=[cfb, gsb], writes=[pqb], skip_self=True)
        do("dve", "tensor_copy", gs[:, 8:12], pq[:, 0:4], reads=[pqb], writes=[gsb])
        do("dve", "tensor_scalar", gs[:, 12:16], gs[:, 8:12], -1.0, None, ALU.mult, reads=[gsb], writes=[gsb])
        do("act", "activation", gs[:, 16:20], gs[:, 8:12], AF.Exp, reads=[gsb], writes=[gsb])
        do("dve", "tensor_tensor", gs[:, 20:24], gs[:, 4:8], gs[:, 16:20], ALU.mult, reads=[gsb], writes=[gsb])
        do("dve", "tensor_scalar", gs[:, 24:28], gs[:, 4:8], -1.0, None, ALU.mult, reads=[gsb], writes=[gsb])
        if getattr(self, "stop_after", None) == "gb": return
        D4, D4b = self.g_D4; gcB, gcBb = self.g_gcB
        for blk in range(4):
            do("dve", "tensor_scalar", D4[:, blk, :], ident, gs[:, 8 + blk:9 + blk], None, ALU.mult, reads=[cfb, gsb], writes=[D4b])
        ps, pb = self.wbk()
        do("pe", "matmul", ps[:], ones, D4[:].rearrange("p a d -> p (a d)"), start=True, stop=True, reads=[cfb, D4b], writes=[pb], skip_self=True)
        do("act", "activation", gcB[:].rearrange("p a d -> p (a d)"), ps[:], AF.Copy, reads=[pb], writes=[gcBb])
        ed, edb = self.g_ed; cd, cdb = self.g_cd
        if getattr(self, "stop_after", None) == "gcb": return
        B_ = self.g_blk
        R = range(4)
        bs = [slice(blk * 128, (blk + 1) * 128) for blk in R]
        nms = self.c_f("negm_strict"); nmi = self.c_f("negm_inclT")
        for blk in R:
            d = B_[blk]
            do("dve", "scalar_tensor_tensor", d["t1"][0][:], gcB[:, blk, :], -1.0, nms, ALU.mult, ALU.add, reads=[gcBb, cfb], writes=[d["t1"][1]])
            do("act", "activation", d["dec"][0][:], d["t1"][0][:], AF.Exp, bias=gs[:, 8 + blk:9 + blk], reads=[d["t1"][1], gsb], writes=[d["dec"][1]])
            do("pool", "tensor_tensor", d["t2"][0][:], gcB[:, blk, :], nmi, ALU.add, reads=[gcBb, cfb], writes=[d["t2"][1]])
            do("act", "activation", d["decT"][0][:], d["t2"][0][:], AF.Exp, bias=gs[:, 12 + blk:13 + blk], reads=[d["t2"][1], gsb], writes=[d["decT"][1]])
            do("act", "activation", d["egcB"][0][:], gcB[:, blk, :], AF.Exp, reads=[gcBb], writes=[d["egcB"][1]])
            do("pool", "tensor_tensor", d["qdT"][0][:], qTn[:, bs[blk]], d["egcB"][0][:], ALU.mult, reads=[qTnb, d["egcB"][1]], writes=[d["qdT"][1]])
            for c in range(2):
                r = slice(64 * c, 64 * c + 64)
                do("act", "activation", ed[r, blk:blk + 1], gcB[r, blk, 64 * c + 63:64 * c + 64], AF.Exp, bias=gs[r, 12 + blk:13 + blk], reads=[gcBb, gsb], writes=[edb])
                do("act", "activation", cd[:, 2 * blk + c:2 * blk + c + 1], gcB[:, blk, 64 * c + 63:64 * c + 64], AF.Exp, reads=[gcBb], writes=[cdb])
            do("dve", "tensor_scalar", d["kdec"][0][:], ktm[:, blk, :], ed[:, blk:blk + 1], None, ALU.mult, reads=[ktmb, edb], writes=[d["kdec"][1]])
            do("dve", "tensor_scalar", d["vb"][0][:], vtm[:, blk, :], gs[:, 4 + blk:5 + blk], None, ALU.mult, reads=[vtmb, gsb], writes=[d["vb"][1]])
            do("dve", "tensor_scalar", d["kbg"][0][:], ktm[:, blk, :], gs[:, 20 + blk:21 + blk], None, ALU.mult, reads=[ktmb, gsb], writes=[d["kbg"][1]])
        if getattr(self, "stop_after", None) == "prep0": return
        pG = self.qg()
        for blk in R:
            do("pe", "matmul", pG[blk][0], kTn[:, bs[blk]], kTn[:, bs[blk]], start=True, stop=True, reads=[kTnb], writes=[pG[blk][1]], skip_self=True)
        for blk in R:
            d = B_[blk]
            do("dve", "scalar_tensor_tensor", d["M"][0][:], pG[blk][0], gs[:, 24 + blk:25 + blk], d["dec"][0][:], ALU.mult, ALU.mult,
               reads=[pG[blk][1], gsb, d["dec"][1]], writes=[d["M"][1]])
        if t == 0:
            self.probe("M0", B_[0]["M"][0], B_[0]["M"][1]); self.probe("dec0", B_[0]["dec"][0], B_[0]["dec"][1]); self.probe("gs", gs, gsb)
            self.probe("kTn", kTn, kTnb); self.probe("gcB", gcB, gcBb); self.probe("ktm", ktm, ktmb)
        import os
        if os.environ.get("GDN_DUMMY") == "1":
            for blk in R:
                d = B_[blk]
                do("dve", "tensor_copy", d["sq"][0][:], d["M"][0][:], reads=[d["M"][1]], writes=[d["sq"][1], d["M"][1]])
        if getattr(self, "stop_after", None) == "pg": return
        pN = self.qg()
        for blk in R:
            d = B_[blk]
            import os
            if os.environ.get("GDN_LHS") == "dec":
                do("pe", "matmul", pN[blk][0], d["dec"][0][:], ident, start=True, stop=True, reads=[d["dec"][1], cfb], writes=[pN[blk][1]], skip_self=True)
            else:
                do("pe", "matmul", pN[blk][0], d["M"][0][:], ident, start=True, stop=True, reads=[d["M"][1], cfb], writes=[pN[blk][1]], skip_self=True)
        import os
        if os.environ.get("GDN_VAR") == "1": return
        for blk in R:
            d = B_[blk]
            do("act", "activation", d["N"][0][:], pN[blk][0], AF.Copy, reads=[pN[blk][1]], writes=[d["N"][1]])
            if os.environ.get("GDN_VAR") == "2": continue
            do("dve", "tensor_tensor", d["X0"][0][:], d["N"][0][:], ident, ALU.add, reads=[d["N"][1], cfb], writes=[d["X0"][1]])
        if getattr(self, "stop_after", None) == "pn": return
        pQ = self.qg()
        for blk in R:
            do("pe", "matmul", pQ[blk][0], kTn[:, bs[blk]], qTn[:, bs[blk]], start=True, stop=True, reads=[kTnb, qTnb], writes=[pQ[blk][1]], skip_self=True)
        for blk in R:
            d = B_[blk]
            do("dve", "tensor_tensor", d["intraT"][0][:], pQ[blk][0], d["decT"][0][:], ALU.mult, reads=[pQ[blk][1], d["decT"][1]], writes=[d["intraT"][1]])
        if getattr(self, "stop_after", None) == "prep1": return
        cur = [{"P": B_[blk]["N"], "PT": B_[blk]["M"], "X": B_[blk]["X0"]} for blk in R]
        for lvl in range(1, 6):
            pp = lvl % 2
            pa = self.qg()
            for blk in R:
                c_ = cur[blk]
                do("pe", "matmul", pa[blk][0], c_["P"][0][:], c_["PT"][0][:], start=True, stop=True, reads=[c_["P"][1], c_["PT"][1]], writes=[pa[blk][1]], skip_self=True)
            pb_ = None
            if lvl < 5:
                pb_ = self.qg()
                for blk in R:
                    c_ = cur[blk]
                    do("pe", "matmul", pb_[blk][0], c_["PT"][0][:], c_["P"][0][:], start=True, stop=True, reads=[c_["P"][1], c_["PT"][1]], writes=[pb_[blk][1]], skip_self=True)
            for blk in R:
                d = B_[blk]
                nPT = d["PT%d" % pp]
                do("act", "activation", nPT[0][:], pa[blk][0], AF.Copy, reads=[pa[blk][1]], writes=[nPT[1]])
                if lvl < 5:
                    nP = d["P%d" % pp]
                    do("dve", "tensor_copy", nP[0][:], pb_[blk][0], reads=[pb_[blk][1]], writes=[nP[1]])
                    cur[blk]["P"] = nP
                cur[blk]["PT"] = nPT
            px = self.qg()
            for blk in R:
                c_ = cur[blk]
                do("pe", "matmul", px[blk][0], c_["PT"][0][:], c_["X"][0][:], start=True, stop=True, reads=[c_["PT"][1], c_["X"][1]], writes=[px[blk][1]], skip_self=True)
            for blk in R:
                d = B_[blk]
                nX = d["X%d" % pp]
                do("dve", "tensor_tensor", nX[0][:], px[blk][0], cur[blk]["X"][0][:], ALU.add, reads=[px[blk][1], cur[blk]["X"][1]], writes=[nX[1]])
                cur[blk]["X"] = nX
        if getattr(self, "stop_after", None) == "neumann": return
        pu = self.qg()
        for blk in R:
            d = B_[blk]; X = cur[blk]["X"]
            do("pe", "matmul", pu[blk][0], X[0][:], d["vb"][0][:], start=True, stop=True, reads=[X[1], d["vb"][1]], writes=[pu[blk][1]], skip_self=True)
        for blk in R:
            d = B_[blk]
            do("act", "activation", d["u"][0][:], pu[blk][0], AF.Copy, reads=[pu[blk][1]], writes=[d["u"][1]])
        pw = self.qg()
        for blk in R:
            d = B_[blk]; X = cur[blk]["X"]
            do("pe", "matmul", pw[blk][0], d["kbg"][0][:], X[0][:], start=True, stop=True, reads=[X[1], d["kbg"][1]], writes=[pw[blk][1]], skip_self=True)
        for blk in R:
            d = B_[blk]
            do("dve", "tensor_copy", d["wT"][0][:], pw[blk][0], reads=[pw[blk][1]], writes=[d["wT"][1]])
        if getattr(self, "stop_after", None) == "uw": return
        Sst, Sstb = self.Sst
        gr, grb = self.g_r
        for blk in R:
            d = B_[blk]
            for c in range(2):
                r = slice(64 * c, 64 * c + 64)
                pA, pAb = self.qb(); pO, pOb = self.qb(); pU, pUb = self.qb()
                do("pe", "matmul", pA, d["wT"][0][:], Sst[:], start=True, stop=True, reads=[d["wT"][1], Sstb], writes=[pAb], skip_self=True)
                do("pe", "matmul", pO, d["qdT"][0][:], Sst[:], start=True, stop=True, reads=[d["qdT"][1], Sstb], writes=[pOb], skip_self=True)
                do("dve", "tensor_tensor", d["vnew"][0][r, :], d["u"][0][r, :], pA[r, :], ALU.subtract, reads=[d["u"][1], pAb], writes=[d["vnew"][1]])
                do("act", "activation", d["ost"][0][r, :], pO[r, :], AF.Copy, reads=[pOb], writes=[d["ost"][1]])
                do("pe", "matmul", pU, d["kdec"][0][r, :], d["vnew"][0][r, :], start=True, stop=True, reads=[d["kdec"][1], d["vnew"][1]], writes=[pUb], skip_self=True)
                do("dve", "scalar_tensor_tensor", Sst[:], Sst[:], cd[:, 2 * blk + c:2 * blk + c + 1], pU, ALU.mult, ALU.add, reads=[Sstb, cdb, pUb], writes=[Sstb])
            pI, pIb = self.qb()
            do("pe", "matmul", pI, d["intraT"][0][:], d["vnew"][0][:], start=True, stop=True, reads=[d["intraT"][1], d["vnew"][1]], writes=[pIb], skip_self=True)
            do("dve", "tensor_tensor", d["o"][0][:], d["ost"][0][:], pI, ALU.add, reads=[d["ost"][1], pIb], writes=[d["o"][1]])
            do("pool", "tensor_tensor", d["sq"][0][:], d["o"][0][:], d["o"][0][:], ALU.mult, reads=[d["o"][1]], writes=[d["sq"][1]])
            do("dve", "tensor_reduce", gr[:, blk:blk + 1], d["sq"][0][:], AX.X, ALU.add, reads=[d["sq"][1]], writes=[grb])
            do("act", "activation", gr[:, 4 + blk:5 + blk], gr[:, blk:blk + 1], AF.Ln, bias=self.epst[0][:, 1:2], scale=1.0 / 128, reads=[grb, self.epst[1]], writes=[grb])
            do("act", "activation", gr[:, 4 + blk:5 + blk], gr[:, 4 + blk:5 + blk], AF.Exp, scale=-0.5, reads=[grb], writes=[grb])
            do("dve", "scalar_tensor_tensor", d["y"][0][:], d["o"][0][:], gr[:, 4 + blk:5 + blk], nwz[:, blk, :], ALU.mult, ALU.mult, reads=[d["o"][1], grb, nwzb], writes=[d["y"][1]])
            t0 = t * TT + blk * 128
            mk.dma("sp", self.ygdn[hs, b, t0:t0 + 128, :], d["y"][0][:], reads=[d["y"][1]], writes=[self.Ygdn], sembuf=d["y"][1])

ALPHA = (2 * 2) ** 0.25

class L2:
    def __init__(self, TC, TT2):
        self.TC, self.TT2 = TC, TT2
        self.NTL = TC // TT2

    def build(self):
        TC, TT2 = self.TC, self.TT2
        nc = bass.Bass("TRN2", target_bir_lowering=False); self.nc = nc
        D = lambda n, s, kind="ExternalInput": nc.dram_tensor(n, s, F32, kind=kind).ap()
        xT = D("xT", [128, 32, TC]); yT = D("yT", [3, 128, 16, TC])
        wgate = D("wgate", [3, 32, 128, 32, 128])
        wbr = D("wbr", [3, 32, 128, 16, 128])
        wout = D("wout", [32, 128, 32, 128])
        lnv = D("lnv", [128, 64])
        ones_d = D("ones", [128, 128])
        x1T = D("x1T", [128, 32, TC], "ExternalOutput")
        with ExitStack() as st:
            mk = MK(nc, st); do = mk.do
            def T(name, shape, dt=F32):
                return st.enter_context(nc.sbuf_tensor(name, shape, dt)), mk.buf(name)
            ps = [st.enter_context(nc.psum_tensor("ps%d" % i, [128, 512], F32)) for i in range(8)]
            pb = [mk.buf("ps%d" % i) for i in range(8)]
            xb_, xbb = T("xb", [128, 32, TT2], BF16)
            yb_ = [T("yb%d" % m, [128, 16, TT2], BF16) for m in range(3)]
            mg, mgb = T("mg", [128, 32, TT2], BF16)
            rT, rTb = T("rT", [128, 32, TT2], F32)
            wg_ = [[T("wg%d_%d" % (m, s), [128, 32, 128], BF16) for s in range(2)] for m in range(3)]
            wb_ = [[T("wb%d_%d" % (m, s), [128, 16, 128], BF16) for s in range(2)] for m in range(3)]
            wo_ = [T("wo%d" % s, [128, 32, 128], BF16) for s in range(2)]
            sg = [T("sg%d" % i, [128, TT2], F32) for i in range(3)]
            acc, accb = T("acc", [128, TT2], F32)
            xr = [T("xr%d" % i, [128, TT2], F32) for i in range(2)]
            sq = [T("sq%d" % i, [128, TT2], F32) for i in range(2)]
            mean, meanb = T("mean", [128, TT2], F32); rstd, rstdb = T("rstd", [128, TT2], F32)
            tmp = [T("tmp%d" % i, [128, TT2], F32) for i in range(2)]
            ot = [T("ot%d" % i, [128, TT2], F32) for i in range(2)]
            lv, lvb = T("lv", [128, 64], F32); on, onb = T("on", [128, 128], F32)
            eps, epsb = T("eps", [128, 1], F32)
            OUT = mk.buf("OUT")
            mk.dma("sp", lv[:], lnv, writes=[lvb]); mk.dma("sp", on[:], ones_d, writes=[onb])
            do("dve", "memset", eps[:], LN_EPS, writes=[epsb])
            wcnt = 0; ocnt = 0
            for tl in range(self.NTL):
                ts = slice(tl * TT2, (tl + 1) * TT2)
                mk.dma("pool", xb_[:], xT[:, :, ts], writes=[xbb])
                for m in range(3):
                    mk.dma("pool", yb_[m][0][:], yT[m, :, :, ts], writes=[yb_[m][1]])
                for n in range(32):
                    s = wcnt % 2; wcnt += 1
                    for m in range(3):
                        mk.dma("pool", wg_[m][s][0][:], wgate[m, n], writes=[wg_[m][s][1]])
                        mk.dma("pool", wb_[m][s][0][:], wbr[m, n], writes=[wb_[m][s][1]])
                    for m in range(3):
                        pg, pgb = ps[m], pb[m]; pr, prb = ps[3 + m], pb[3 + m]
                        w, wb2 = wg_[m][s]
                        for k in range(32):
                            do("pe", "matmul", pg[:, 0:TT2], w[:, k, :], xb_[:, k, :], start=(k == 0), stop=(k == 31), reads=[wb2, xbb], writes=[pgb], skip_self=True)
                        w, wb2 = wb_[m][s]
                        for k in range(16):
                            do("pe", "matmul", pr[:, 0:TT2], w[:, k, :], yb_[m][0][:, k, :], start=(k == 0), stop=(k == 15), reads=[wb2, yb_[m][1]], writes=[prb], skip_self=True)
                        g, gb = sg[m]
                        do("act", "activation", g[:], pg[:, 0:TT2], AF.Exp, scale=-1.0, reads=[pgb], writes=[gb])
                        do("pool", "tensor_scalar", g[:], g[:], 1.0, None, ALU.add, reads=[gb], writes=[gb])
                        do("dve", "reciprocal", g[:], g[:], reads=[gb], writes=[gb])
                        if m == 0:
                            do("dve", "tensor_tensor", acc[:], pr[:, 0:TT2], g[:], ALU.mult, reads=[prb, gb], writes=[accb])
                        else:
                            do("dve", "tensor_tensor", g[:], pr[:, 0:TT2], g[:], ALU.mult, reads=[prb, gb], writes=[gb])
                            if m == 1:
                                do("pool", "tensor_tensor", acc[:], acc[:], g[:], ALU.add, reads=[accb, gb], writes=[accb])
                            else:
                                do("pool", "tensor_tensor", mg[:, n, :], acc[:], g[:], ALU.add, reads=[accb, gb], writes=[mgb])
                for e in range(32):
                    s = wcnt % 2; wcnt += 1
                    w, wb2 = wo_[s]
                    mk.dma("pool", w[:], wout[e], writes=[wb2])
                    x_, x_b = xr[e % 2]
                    mk.dma("sp", x_[:], xT[:, e, ts], writes=[x_b])
                    ph, phb = ps[6 + e % 2], pb[6 + e % 2]
                    for n in range(32):
                        do("pe", "matmul", ph[:, 0:TT2], w[:, n, :], mg[:, n, :], start=(n == 0), stop=(n == 31), reads=[wb2, mgb], writes=[phb], skip_self=True)
                    do("dve", "scalar_tensor_tensor", rT[:, e, :], x_[:], ALPHA, ph[:, 0:TT2], ALU.mult, ALU.add, reads=[x_b, phb], writes=[rTb])
                self.layer_norm(mk, rT, rTb, ps, pb, on, onb, sq, mean, meanb, rstd, rstdb, tmp, eps, epsb, lv, lvb, ot, x1T, ts, OUT, TT2)
            mk.finish([OUT])
        return nc

    @staticmethod
    def layer_norm(mk, rT, rTb, ps, pb, on, onb, sq, mean, meanb, rstd, rstdb, tmp, eps, epsb, lv, lvb, ot, outT, ts, OUT, TT2):
        do = mk.do
        p1, p1b = ps[0], pb[0]; p2, p2b = ps[1], pb[1]
        for e in range(32):
            do("pe", "matmul", p1[:, 0:TT2], on[:], rT[:, e, :], start=(e == 0), stop=(e == 31), reads=[onb, rTb], writes=[p1b], skip_self=True)
        for e in range(32):
            q, qb = sq[e % 2]
            do("act", "activation", q[:], rT[:, e, :], AF.Square, reads=[rTb], writes=[qb])
            do("pe", "matmul", p2[:, 0:TT2], on[:], q[:], start=(e == 0), stop=(e == 31), reads=[onb, qb], writes=[p2b], skip_self=True)
        t0, t0b = tmp[0]
        do("dve", "tensor_scalar", mean[:], p1[:, 0:TT2], 1.0 / 4096, None, ALU.mult, reads=[p1b], writes=[meanb])
        do("dve", "tensor_tensor", t0[:], mean[:], mean[:], ALU.mult, reads=[meanb], writes=[t0b])
        do("dve", "scalar_tensor_tensor", t0[:], p2[:, 0:TT2], 1.0 / 4096, t0[:], ALU.mult, ALU.subtract, reads=[p2b, t0b], writes=[t0b])
        do("act", "activation", rstd[:], t0[:], AF.Ln, bias=eps[:, 0:1], reads=[t0b, epsb], writes=[rstdb])
        do("act", "activation", rstd[:], rstd[:], AF.Exp, scale=-0.5, reads=[rstdb], writes=[rstdb])
        for e in range(32):
            t1, t1b = tmp[1] if e % 2 else tmp[0]
            o, ob = ot[e % 2]
            do("pool", "tensor_tensor", t1[:], rT[:, e, :], mean[:], ALU.subtract, reads=[rTb, meanb], writes=[t1b])
            do("pool", "tensor_tensor", t1[:], t1[:], rstd[:], ALU.mult, reads=[t1b, rstdb], writes=[t1b])
            do("dve", "tensor_scalar", o[:], t1[:], lv[:, e:e + 1], lv[:, 32 + e:33 + e], ALU.mult, ALU.add, reads=[t1b, lvb], writes=[ob])
            mk.dma("sp", outT[:, e, ts], o[:], reads=[ob], writes=[OUT], sembuf=ob)


def make_sel():
    sel = np.zeros((32, 32 * 128), np.float32)
    for e in range(32):
        sel[e, e * 128:(e + 1) * 128] = 1.0
    return sel

class L3:
    def __init__(self, TC, TT2):
        self.TC, self.TT2 = TC, TT2
        self.NTL = TC // TT2

    def build(self):
        TC, TT2 = self.TC, self.TT2
        nb = TT2 // 128
        nc = bass.Bass("TRN2", target_bir_lowering=False); self.nc = nc
        D = lambda n, s, kind="ExternalInput": nc.dram_tensor(n, s, F32, kind=kind).ap()
        x1T = D("x1T", [128, 32, TC])
        wr_d = D("wr", [128, 32, 36]); rb_d = D("rb", [128, 36])
        ident_d = D("ident", [128, 128]); ones_d = D("ones", [128, 128])
        wg_d = D("wg", [32, 4, 128, 32, 128]); wu_d = D("wu", [32, 4, 128, 32, 128]); wd_d = D("wd", [32, 128, 4, 4096])
        lnv = D("lnv", [128, 64])
        x2T = D("x2T", [128, 32, TC], "ExternalOutput")
        with ExitStack() as st:
            mk = MK(nc, st); do = mk.do
            def T(name, shape, dt=F32):
                return st.enter_context(nc.sbuf_tensor("s_" + name, shape, dt)), mk.buf(name)
            ps = [st.enter_context(nc.psum_tensor("ps%d" % i, [128, 512], F32)) for i in range(8)]
            pb = [mk.buf("ps%d" % i) for i in range(8)]
            xfk = [T("xfk%d" % i, [128, 32, 128], F32) for i in range(1)]
            xb_, xbb = T("xb", [128, 32, TT2], BF16)
            xr = [T("xr%d" % i, [128, TT2], F32) for i in range(2)]
            sl, slb = T("sl", [32, 128], F32)
            acc, accb = T("acc", [128, 32, TT2], F32)
            wg_ = [T("wg%d" % s, [128, 32, 128], BF16) for s in range(2)]
            wu_ = [T("wu%d" % s, [128, 32, 128], BF16) for s in range(2)]
            wd_ = [T("wd%d" % s, [128, 4, 1024], BF16) for s in range(2)]
            hT, hTb = T("hT", [128, 4, TT2], BF16)
            wr, wrb = T("wr", [128, 32, 36], F32); rb, rbb = T("rb", [128, 36], F32)
            ident, identb = T("ident", [128, 128], F32); on, onb = T("on", [128, 128], F32)
            lv, lvb = T("lv", [128, 64], F32); eps, epsb = T("eps", [128, 1], F32)
            lg, lgb = T("lg", [128, 36], F32)
            sm, smb = T("sm", [128, 96], F32)
            comb, combb = T("comb", [128, 32], F32)
            cT, cTb = T("cT", [32, TC], F32)
            cB, cBb = T("cB", [128, TT2], F32)
            e1 = [T("e1_%d" % i, [128, TT2], F32) for i in range(2)]
            sq = [T("sq%d" % i, [128, TT2], F32) for i in range(2)]
            mean, meanb = T("mean", [128, TT2], F32); rstd, rstdb = T("rstd", [128, TT2], F32)
            tmp = [T("tmp%d" % i, [128, TT2], F32) for i in range(2)]
            ot = [T("ot%d" % i, [128, TT2], F32) for i in range(2)]
            OUT = mk.buf("OUT")
            for (t_, tb_, d_) in ((wr, wrb, wr_d), (rb, rbb, rb_d), (ident, identb, ident_d), (on, onb, ones_d), (lv, lvb, lnv)):
                mk.dma("sp", t_[:], d_, writes=[tb_])
            do("dve", "memset", eps[:], LN_EPS, writes=[epsb])
            wc = 0; dc = 0
            for blk in range(TC // 128):
                bsl = slice(blk * 128, (blk + 1) * 128)
                p0, p0b = ps[0], pb[0]
                xf, xfb = xfk[0]
                mk.dma("sp", xf[:], x1T[:, :, bsl], writes=[xfb])
                for k in range(32):
                    do("pe", "matmul", p0[:, 0:36], xf[:, k, :], wr[:, k, :], start=(k == 0), stop=(k == 31), reads=[xfb, wrb], writes=[p0b], skip_self=True)
                do("dve", "tensor_tensor", lg[:], p0[:, 0:36], rb[:], ALU.add, reads=[p0b, rbb], writes=[lgb])
                S_ = lambda a, b_: sm[:, a:b_]
                R1 = dict(reads=[smb, lgb], writes=[smb])
                do("dve", "tensor_reduce", S_(0, 1), lg[:, 0:4], AX.X, ALU.max, **R1)
                do("dve", "tensor_scalar", S_(1, 2), S_(0, 1), -1.0, None, ALU.mult, **R1)
                do("dve", "tensor_scalar", S_(2, 6), lg[:, 0:4], S_(0, 1), None, ALU.is_ge, **R1)
                do("act", "activation", S_(6, 10), lg[:, 0:4], AF.Exp, bias=S_(1, 2), **R1)
                do("dve", "tensor_reduce", S_(10, 11), S_(6, 10), AX.X, ALU.add, **R1)
                do("dve", "reciprocal", S_(11, 12), S_(10, 11), **R1)
                do("dve", "tensor_scalar", S_(12, 20), lg[:, 4:12], S_(2, 3), None, ALU.mult, **R1)
                for g in range(1, 4):
                    do("dve", "scalar_tensor_tensor", S_(12, 20), lg[:, 4 + 8 * g:12 + 8 * g], S_(2 + g, 3 + g), S_(12, 20), ALU.mult, ALU.add, **R1)
                do("dve", "tensor_reduce", S_(20, 21), S_(12, 20), AX.X, ALU.max, **R1)
                do("dve", "tensor_scalar", S_(21, 29), S_(12, 20), S_(20, 21), None, ALU.is_ge, **R1)
                do("dve", "scalar_tensor_tensor", S_(29, 37), S_(21, 29), -1e30, S_(12, 20), ALU.mult, ALU.add, **R1)
                do("dve", "tensor_reduce", S_(37, 38), S_(29, 37), AX.X, ALU.max, **R1)
                do("dve", "tensor_scalar", S_(38, 46), S_(29, 37), S_(37, 38), None, ALU.is_ge, **R1)
                do("dve", "tensor_tensor", S_(46, 47), S_(37, 38), S_(20, 21), ALU.subtract, **R1)
                do("act", "activation", S_(47, 48), S_(46, 47), AF.Exp, **R1)
                do("dve", "tensor_scalar", S_(48, 49), S_(47, 48), 1.0, None, ALU.add, **R1)
                do("dve", "reciprocal", S_(49, 50), S_(48, 49), **R1)
                do("dve", "tensor_tensor", S_(50, 51), S_(49, 50), S_(11, 12), ALU.mult, **R1)
                do("dve", "tensor_tensor", S_(51, 52), S_(50, 51), S_(47, 48), ALU.mult, **R1)
                do("dve", "tensor_scalar", S_(52, 60), S_(21, 29), S_(50, 51), None, ALU.mult, **R1)
                do("dve", "scalar_tensor_tensor", S_(52, 60), S_(38, 46), S_(51, 52), S_(52, 60), ALU.mult, ALU.add, **R1)
                for g in range(4):
                    do("dve", "tensor_scalar", comb[:, 8 * g:8 * g + 8], S_(52, 60), S_(2 + g, 3 + g), None, ALU.mult, reads=[smb], writes=[combb])
                p1, p1b = ps[1], pb[1]
                do("pe", "matmul", p1[0:32, 0:128], comb[:], ident[:], start=True, stop=True, reads=[combb, identb], writes=[p1b], skip_self=True)
                do("act", "activation", cT[:, bsl], p1[0:32, 0:128], AF.Copy, reads=[p1b], writes=[cTb])
            for tl in range(self.NTL):
                ts = slice(tl * TT2, (tl + 1) * TT2)
                mk.dma("pool", xb_[:], x1T[:, :, ts], writes=[xbb])
                for e in range(32):
                    p1, p1b = ps[1], pb[1]
                    do("dve", "tensor_scalar", sl[:], on[0:32, :], ident[0:32, e:e + 1], None, ALU.mult, reads=[onb, identb], writes=[slb])
                    do("pe", "matmul", p1[:, 0:TT2], sl[:], cT[:, ts], start=True, stop=True, reads=[slb, cTb], writes=[p1b], skip_self=True)
                    do("act", "activation", cB[:], p1[:, 0:TT2], AF.Copy, reads=[p1b], writes=[cBb])
                    for f in range(4):
                        s = wc % 2; wc += 1
                        wg, wgb = wg_[s]; wu, wub = wu_[s]
                        mk.dma("pool", wg[:], wg_d[e, f], writes=[wgb])
                        mk.dma("pool", wu[:], wu_d[e, f], writes=[wub])
                        pg, pgb = ps[2 + f % 2], pb[2 + f % 2]; pu, pub = ps[4 + f % 2], pb[4 + f % 2]
                        for k in range(32):
                            do("pe", "matmul", pg[:, 0:TT2], wg[:, k, :], xb_[:, k, :], start=(k == 0), stop=(k == 31), reads=[wgb, xbb], writes=[pgb], skip_self=True)
                        for k in range(32):
                            do("pe", "matmul", pu[:, 0:TT2], wu[:, k, :], xb_[:, k, :], start=(k == 0), stop=(k == 31), reads=[wub, xbb], writes=[pub], skip_self=True)
                        t1, t1b = e1[f % 2]
                        do("act", "activation", t1[:], pg[:, 0:TT2], AF.Exp, scale=-1.0, reads=[pgb], writes=[t1b])
                        do("pool", "tensor_scalar", t1[:], t1[:], 1.0, None, ALU.add, reads=[t1b], writes=[t1b])
                        do("dve", "reciprocal", t1[:], t1[:], reads=[t1b], writes=[t1b])
                        do("dve", "tensor_tensor", t1[:], pg[:, 0:TT2], t1[:], ALU.mult, reads=[pgb, t1b], writes=[t1b])
                        do("dve", "tensor_tensor", t1[:], pu[:, 0:TT2], t1[:], ALU.mult, reads=[pub, t1b], writes=[t1b])
                        do("pool", "tensor_tensor", hT[:, f, :], t1[:], cB[:], ALU.mult, reads=[t1b, cBb], writes=[hTb])
                    for half in range(4):
                        s = dc % 2; dc += 1
                        wd, wdb = wd_[s]
                        mk.dma("pool", wd[:], wd_d[e, :, :, half * 1024:(half + 1) * 1024], writes=[wdb])
                        for cc in range(8):
                            c = half * 8 + cc
                            po, pob = ps[6 + c % 2], pb[6 + c % 2]
                            for f in range(4):
                                do("pe", "matmul", po[:, 0:TT2], wd[:, f, cc * 128:(cc + 1) * 128], hT[:, f, :], start=(f == 0), stop=(f == 3), reads=[wdb, hTb], writes=[pob], skip_self=True)
                            if e == 0:
                                do("dve", "tensor_copy", acc[:, c, :], po[:, 0:TT2], reads=[pob], writes=[accb])
                            else:
                                do("dve", "tensor_tensor", acc[:, c, :], acc[:, c, :], po[:, 0:TT2], ALU.add, reads=[pob, accb], writes=[accb])
                for c in range(32):
                    x_, x_b = xr[c % 2]
                    mk.dma("sp", x_[:], x1T[:, c, ts], writes=[x_b])
                    do("dve", "scalar_tensor_tensor", acc[:, c, :], x_[:], ALPHA, acc[:, c, :], ALU.mult, ALU.add, reads=[x_b, accb], writes=[accb])
                L2.layer_norm(mk, acc, accb, ps, pb, on, onb, sq, mean, meanb, rstd, rstdb, tmp, eps, epsb, lv, lvb, ot, x2T, ts, OUT, TT2)
            mk.finish([OUT])
        return nc


_PROGS = {}

def _prog(key, fn):
    if key not in _PROGS:
        _PROGS[key] = fn()
    return _PROGS[key]

def _wtile(W):
    n = W.shape[1]
    return np.ascontiguousarray(np.asarray(W).reshape(32, 128, n).transpose(1, 0, 2))

def _wt(W, kchunks, nchunks):
    return np.ascontiguousarray(np.asarray(W).reshape(kchunks, 128, nchunks, 128).transpose(2, 1, 0, 3))

def _l1_inputs(inp, l, core, L, xT_all, S):
    w_in = inp["w_in"][l]
    m = {"xT": xT_all}
    hh = [2 * core, 2 * core + 1]
    kinds = L.kinds
    if "sba" in kinds:
        m["wsba"] = np.stack([np.stack([_wtile(w_in[:, o + h * 128: o + (h + 1) * 128]) for o in (0, 2048, 4096)]) for h in hh])
    if "diff" in kinds:
        m["wdiff"] = np.stack([np.stack([_wtile(w_in[:, 14368 + o + h * 128: 14368 + o + (h + 1) * 128]) for o in (0, 2048, 4096)]) for h in hh])
        cosT, sinS = make_rope(S)
        m["ropec"] = cosT; m["ropes"] = sinS
    if "gdn" in kinds:
        m["wgdn"] = np.stack([np.stack([_wtile(w_in[:, o + h * 128: o + (h + 1) * 128]) for o in (6144, 8192, 10240, 12288)]) for h in hh])
        ab = np.stack([w_in[:, 14336 + hh[0]], w_in[:, 14352 + hh[0]], w_in[:, 14336 + hh[1]], w_in[:, 14352 + hh[1]]], axis=1)
        m["wgab"] = _wtile(ab)
    vec = np.zeros((128, 1024), np.float32)
    vec[:, 0:64] = inp["diff_lambda_q1"][l][None, :]; vec[:, 64:128] = inp["diff_lambda_k1"][l][None, :]
    vec[:, 128:192] = inp["diff_lambda_q2"][l][None, :]; vec[:, 192:256] = inp["diff_lambda_k2"][l][None, :]
    lam_init = 0.8 - 0.6 * math.exp(-0.3 * l)
    vec[:, 256] = lam_init; vec[:, 257] = 1.0 - lam_init
    vec[:, 258] = inp["diff_subln_w"][l]
    cw = inp["conv_w"][l]
    for hs, h in enumerate(hh):
        for mi, o in enumerate((0, 2048, 4096)):
            vec[:, 260 + (hs * 3 + mi) * 4: 260 + (hs * 3 + mi) * 4 + 4] = cw[:, o + h * 128: o + (h + 1) * 128].T
        vec[:, 290 + hs] = inp["gdn_a_log"][l][h]; vec[:, 292 + hs] = inp["gdn_dt_bias"][l][h]
    vec[:, 300:428] = inp["gdn_norm_w"][l][None, :]
    m["vec"] = vec
    m["cstf"] = L.cstf_np; m["cstb"] = L.cstb_np
    return m

def _l2_weights(inp, l):
    w_in = inp["w_in"][l]
    m = {}
    m["wgate"] = np.stack([_wt(w_in[:, 20512 + i * 4096: 20512 + (i + 1) * 4096], 32, 32) for i in range(3)])
    m["wbr"] = np.stack([_wt(inp[k][l], 16, 32) for k in ("w_branch_sba", "w_branch_gdn", "w_branch_diff")])
    m["wout"] = _wt(inp["w_out"][l], 32, 32)
    lnv = np.zeros((128, 64), np.float32)
    lnv[:, 0:32] = np.asarray(inp["ln1_g"][l]).reshape(32, 128).T; lnv[:, 32:64] = np.asarray(inp["ln1_b"][l]).reshape(32, 128).T
    m["lnv"] = lnv; m["ones"] = np.ones((128, 128), np.float32)
    return m

def _l3_weights(inp, l):
    m = {}
    wr = np.concatenate([np.asarray(inp["w_router_group"][l]), np.asarray(inp["w_router_expert"][l])], axis=1)
    m["wr"] = np.ascontiguousarray(wr.reshape(32, 128, 36).transpose(1, 0, 2))
    rb = np.concatenate([np.asarray(inp["b_router_group"][l]), np.asarray(inp["b_router_expert"][l])])
    m["rb"] = np.ascontiguousarray(np.broadcast_to(rb[None, :], (128, 36))).astype(np.float32)
    m["ident"] = np.eye(128, dtype=np.float32); m["ones"] = np.ones((128, 128), np.float32)
    def gu(W):
        return np.ascontiguousarray(np.asarray(W).reshape(32, 32, 128, 4, 128).transpose(0, 3, 2, 1, 4))
    m["wg"] = gu(inp["w_expert_gate"][l]); m["wu"] = gu(inp["w_expert_up"][l])
    m["wd"] = np.ascontiguousarray(np.asarray(inp["w_expert_down"][l]).reshape(32, 4, 128, 4096).transpose(0, 2, 1, 3))
    lnv = np.zeros((128, 64), np.float32)
    lnv[:, 0:32] = np.asarray(inp["ln2_g"][l]).reshape(32, 128).T; lnv[:, 32:64] = np.asarray(inp["ln2_b"][l]).reshape(32, 128).T
    m["lnv"] = lnv
    return m

def kernel(**inputs):
    inp = {k: np.asarray(v) for k, v in inputs.items()}
    x = inp["x"]
    B, S, Dm = x.shape
    NCORES = 8
    TC = B * S // NCORES
    per_b = S // TC
    TT2 = 256
    cores = list(range(NCORES))
    xT_all = np.ascontiguousarray(x.transpose(0, 2, 1).reshape(B, 32, 128, S).transpose(0, 2, 1, 3))
    for l in range(2):
        ua = [(k, hs, b) for k in ("sba", "diff") for hs in range(2) for b in range(B)]
        ub = [("gdn", hs, b) for hs in range(2) for b in range(B)]
        La = _prog(("l1a", S, B), lambda: (lambda L: (L, L.build()))(L1(S, B, ua)))
        Lb = _prog(("l1b", S, B), lambda: (lambda L: (L, L.build()))(L1(S, B, ub)))
        ra = run_bass_kernel_spmd(La[1], [_l1_inputs(inp, l, c, La[0], xT_all, S) for c in cores], core_ids=cores).results
        rb = run_bass_kernel_spmd(Lb[1], [_l1_inputs(inp, l, c, Lb[0], xT_all, S) for c in cores], core_ids=cores).results
        P2 = _prog(("l2", TC, TT2), lambda: L2(TC, TT2).build())
        w2 = _l2_weights(inp, l)
        ims = []
        for c in cores:
            b = c // per_b; s0 = (c % per_b) * TC
            yT = np.empty((3, 128, 16, TC), np.float32)
            for h in range(16):
                yT[0][:, h, :] = ra[h // 2]["ysba"][h % 2, b, :, s0:s0 + TC]
                yT[1][:, h, :] = rb[h // 2]["ygdn"][h % 2, b, s0:s0 + TC, :].T
                yT[2][:, h, :] = ra[h // 2]["ydiff"][h % 2, b, :, s0:s0 + TC]
            m = dict(w2); m["xT"] = np.ascontiguousarray(xT_all[b, :, :, s0:s0 + TC]); m["yT"] = yT
            ims.append(m)
        r2 = run_bass_kernel_spmd(P2, ims, core_ids=cores).results
        del ims, ra, rb, w2
        TT3 = 512 if TC % 512 == 0 else 256
        P3 = _prog(("l3", TC, TT3), lambda: L3(TC, TT3).build())
        w3 = _l3_weights(inp, l)
        ims = []
        for c in cores:
            m = dict(w3); m["x1T"] = r2[c]["x1T"]; ims.append(m)
        r3 = run_bass_kernel_spmd(P3, ims, core_ids=cores).results
        del ims, w3, r2
        xT_all = np.empty((B, 128, 32, S), np.float32)
        for c in cores:
            b = c // per_b; s0 = (c % per_b) * TC
            xT_all[b, :, :, s0:s0 + TC] = r3[c]["x2T"]
        del r3
    out = np.ascontiguousarray(xT_all.transpose(0, 3, 2, 1).reshape(B, S, Dm))
    return out.astype(np.float32)
```
